# Optimizing a Trainium2 kernel written in Bass

```python
import math
import jax
import jax.numpy as jnp
from jax import lax
import numpy as np

D_MODEL = 1024
BATCH = 8
SEQ = 2048
DEPTH = 1

PLE_DIM = 256
POOL_WIDTH = D_MODEL // 2
POOL_GROUPS = 4
POOL_GROUP_DIM = POOL_WIDTH // POOL_GROUPS
POOL_WINDOWS = (2, 4, 8, 16)
SB_HEAD_DIM = 64
SB_HEADS = (D_MODEL // 2) // SB_HEAD_DIM
SB_WIDTH = SB_HEADS * SB_HEAD_DIM
Q_BLOCK = 128
N_BRANCHES = 2
OFF_Q = POOL_WIDTH
OFF_K = OFF_Q + SB_WIDTH
OFF_V = OFF_K + SB_WIDTH
OFF_GATE = OFF_V + SB_WIDTH
IN_WIDTH = OFF_GATE + N_BRANCHES * D_MODEL
N_GROUPS = 4
EXPERTS_PER_GROUP = 8
TOP_K_IN_GROUP = 2
D_EXPERT = D_MODEL // 2
LN_EPS = 1e-5
DEEPNORM_ALPHA = (2.0 * DEPTH) ** 0.25
DEEPNORM_BETA = (8.0 * DEPTH) ** -0.25

kernel_name = "hybrid_pool_stickbreak_hmoe_deepnorm"


def layer_norm(x, g, b):
    xf = x.astype(jnp.float32)
    mu = jnp.mean(xf, axis=-1, keepdims=True)
    var = jnp.mean(jnp.square(xf - mu), axis=-1, keepdims=True)
    y = (xf - mu) * lax.rsqrt(var + LN_EPS)
    return (y * g.astype(jnp.float32) + b.astype(jnp.float32)).astype(x.dtype)


def pool_mixer(u, w_pool, pool_scale):
    bsz, seq, _ = u.shape
    uf = u.astype(jnp.float32)
    csum = jnp.pad(jnp.cumsum(uf, axis=1), ((0, 0), (1, 0), (0, 0)))
    pos = jnp.arange(1, seq + 1, dtype=jnp.float32)
    outs = []
    for g, w in enumerate(POOL_WINDOWS):
        sl = slice(g * POOL_GROUP_DIM, (g + 1) * POOL_GROUP_DIM)
        cg = csum[..., sl]
        lo = jnp.pad(cg, ((0, 0), (w - 1, 0), (0, 0)))[:, :seq]
        cnt = jnp.minimum(pos, float(w))[None, :, None]
        outs.append((cg[:, 1:] - lo) / cnt - uf[..., sl])
    pooled = jnp.stack(outs, axis=2).astype(u.dtype)
    mixed = jnp.einsum('bsgc,gcd->bsgd', pooled, w_pool).reshape(bsz, seq, POOL_WIDTH)
    return mixed * pool_scale


def stick_breaking_attention(q, k, v):
    seq = q.shape[2]
    scale = 1.0 / math.sqrt(q.shape[-1])
    outs = []
    for qb in range(seq // Q_BLOCK):
        start = qb * Q_BLOCK
        end = start + Q_BLOCK
        qf = q[:, :, start:end].astype(jnp.float32)
        kf = k[:, :, :end].astype(jnp.float32)
        z = jnp.einsum('bhqd,bhkd->bhqk', qf, kf) * scale
        qpos = jnp.arange(start, end)
        kpos = jnp.arange(end)
        causal = kpos[None, :] < qpos[:, None]
        log_beta = jax.nn.log_sigmoid(z)
        log_rem = jnp.where(causal, jax.nn.log_sigmoid(-z), 0.0)
        later = lax.cumsum(log_rem, axis=3, reverse=True) - log_rem
        a = jnp.where(causal, jnp.exp(log_beta + later), 0.0)
        outs.append(jnp.einsum('bhqk,bhkd->bhqd', a.astype(v.dtype), v[:, :, :end]))
    return jnp.concatenate(outs, axis=2)


def hierarchical_moe(x, w_rg, b_rg, w_re, b_re, w_eg, w_eu, w_ed):
    bsz, seq, d = x.shape
    xt = x.reshape(-1, d)
    xf = xt.astype(jnp.float32)
    group_probs = jax.nn.softmax(xf @ w_rg.astype(jnp.float32) + b_rg.astype(jnp.float32), axis=-1)
    g_idx = jnp.argmax(group_probs, axis=-1)
    g_prob = jnp.take_along_axis(group_probs, g_idx[:, None], axis=1)[:, 0]
    expert_logits = jnp.einsum('td,gde->tge', xf, w_re.astype(jnp.float32)) + b_re.astype(jnp.float32)
    sel_logits = jnp.take_along_axis(expert_logits, g_idx[:, None, None], axis=1)[:, 0]
    top_vals, top_idx = lax.top_k(sel_logits, TOP_K_IN_GROUP)
    top_w = jax.nn.softmax(top_vals, axis=-1) * g_prob[:, None]
    within = jnp.sum(jax.nn.one_hot(top_idx, EXPERTS_PER_GROUP, dtype=jnp.float32) * top_w[..., None], axis=1)
    combine = (jax.nn.one_hot(g_idx, N_GROUPS, dtype=jnp.float32)[:, :, None] * within[:, None, :]).astype(x.dtype)
    y = jnp.zeros_like(xt)
    for g in range(N_GROUPS):
        for e in range(EXPERTS_PER_GROUP):
            h = jax.nn.silu(xt @ w_eg[g, e]) * (xt @ w_eu[g, e])
            y = y + (h @ w_ed[g, e]) * combine[:, g, e, None]
    return y.reshape(bsz, seq, d)


def setup_inputs(seed: int = 0) -> dict:
    key = jax.random.key(seed)
    ks = jax.random.split(key, 24)
    L, D = DEPTH, D_MODEL

    def nrm(k, shape, scale):
        return jax.random.normal(k, shape, jnp.float32) * scale

    col_scale = jnp.concatenate([
        jnp.ones((POOL_WIDTH + 2 * SB_WIDTH,), jnp.float32),
        jnp.full((SB_WIDTH,), DEEPNORM_BETA, jnp.float32),
        jnp.ones((N_BRANCHES * D,), jnp.float32)])
    return {
        "x": nrm(ks[0], (BATCH, SEQ, D), 1.0),
        "p": nrm(ks[1], (DEPTH, BATCH, SEQ, PLE_DIM), 1.0),
        "w_in": nrm(ks[2], (L, D, IN_WIDTH), D ** -0.5) * col_scale,
        "w_pool": nrm(ks[3], (L, POOL_GROUPS, POOL_GROUP_DIM, POOL_GROUP_DIM), POOL_GROUP_DIM ** -0.5),
        "pool_scale": 1.0 + nrm(ks[4], (L, POOL_WIDTH), 0.1),
        "w_pu": nrm(ks[5], (L, POOL_WIDTH, D), DEEPNORM_BETA * POOL_WIDTH ** -0.5),
        "w_au": nrm(ks[6], (L, SB_WIDTH, D), DEEPNORM_BETA * SB_WIDTH ** -0.5),
        "w_o": nrm(ks[7], (L, D, D), DEEPNORM_BETA * D ** -0.5),
        "ln1_g": 1.0 + nrm(ks[8], (L, D), 0.02),
        "ln1_b": nrm(ks[9], (L, D), 0.02),
        "w_rg": nrm(ks[10], (L, D, N_GROUPS), D ** -0.5),
        "b_rg": nrm(ks[11], (L, N_GROUPS), 0.01),
        "w_re": nrm(ks[12], (L, N_GROUPS, D, EXPERTS_PER_GROUP), D ** -0.5),
        "b_re": nrm(ks[13], (L, N_GROUPS, EXPERTS_PER_GROUP), 0.01),
        "w_eg": nrm(ks[14], (L, N_GROUPS, EXPERTS_PER_GROUP, D, D_EXPERT), DEEPNORM_BETA * D ** -0.5),
        "w_eu": nrm(ks[15], (L, N_GROUPS, EXPERTS_PER_GROUP, D, D_EXPERT), DEEPNORM_BETA * D ** -0.5),
        "w_ed": nrm(ks[16], (L, N_GROUPS, EXPERTS_PER_GROUP, D_EXPERT, D), DEEPNORM_BETA * D_EXPERT ** -0.5),
        "w_pg": nrm(ks[17], (L, D, D), D ** -0.5),
        "w_pp": nrm(ks[18], (L, PLE_DIM, D), DEEPNORM_BETA * PLE_DIM ** -0.5),
        "ln2_g": 1.0 + nrm(ks[19], (L, D), 0.02),
        "ln2_b": nrm(ks[20], (L, D), 0.02),
    }


def reference(x, p, w_in, w_pool, pool_scale, w_pu, w_au, w_o, ln1_g, ln1_b,
              w_rg, b_rg, w_re, b_re, w_eg, w_eu, w_ed, w_pg, w_pp, ln2_g, ln2_b):
    bsz, seq, d = x.shape
    for i in range(DEPTH):
        proj = x @ w_in[i]
        u_pool = proj[..., :OFF_Q]
        q = proj[..., OFF_Q:OFF_K].reshape(bsz, seq, SB_HEADS, SB_HEAD_DIM).transpose(0, 2, 1, 3)
        k = proj[..., OFF_K:OFF_V].reshape(bsz, seq, SB_HEADS, SB_HEAD_DIM).transpose(0, 2, 1, 3)
        v = proj[..., OFF_V:OFF_GATE].reshape(bsz, seq, SB_HEADS, SB_HEAD_DIM).transpose(0, 2, 1, 3)
        gates = jax.nn.sigmoid(proj[..., OFF_GATE:].astype(jnp.float32)).astype(x.dtype)
        gates = gates.reshape(bsz, seq, N_BRANCHES, d)

        pool_out = pool_mixer(u_pool, w_pool[i], pool_scale[i])
        attn_out = stick_breaking_attention(q, k, v).transpose(0, 2, 1, 3).reshape(bsz, seq, SB_WIDTH)

        merged = gates[:, :, 0] * (pool_out @ w_pu[i]) + gates[:, :, 1] * (attn_out @ w_au[i])
        x = layer_norm(DEEPNORM_ALPHA * x + merged @ w_o[i], ln1_g[i], ln1_b[i])

        moe_out = hierarchical_moe(x, w_rg[i], b_rg[i], w_re[i], b_re[i], w_eg[i], w_eu[i], w_ed[i])
        ple = jax.nn.sigmoid(x @ w_pg[i]) * (p[i] @ w_pp[i])
        x = layer_norm(DEEPNORM_ALPHA * x + moe_out + ple, ln2_g[i], ln2_b[i])
    return x
```

```python
import numpy as np
from contextlib import ExitStack
import concourse.bass as bass
import concourse.mybir as mybir
from concourse.bass_utils import run_bass_kernel_spmd

F32 = mybir.dt.float32
BF16 = mybir.dt.bfloat16
AF = mybir.ActivationFunctionType
ALU = mybir.AluOpType
AX = mybir.AxisListType

S = 2048
D = 1024
NT = S // 128
ALPHA = 2.0 ** 0.25
EPS = 1e-5
NEXP = 32


class Op:
    __slots__ = ("eng", "fn", "idx", "deps", "milestone", "semval")


class Prog:
    ENGS = ("pe", "act", "dve", "pool", "sp")

    def __init__(self):
        self.ops = {e: [] for e in self.ENGS}
        self.last_writer = {}
        self.readers = {}
        self.enabled = True

    def add(self, eng, fn, reads=(), writes=()):
        if not self.enabled:
            return None
        op = Op()
        op.eng = eng
        op.fn = fn
        op.idx = len(self.ops[eng])
        op.milestone = False
        op.semval = None
        deps = set()
        reads = list(reads) + ["_bar"]
        for b in reads:
            w = self.last_writer.get(b)
            if w is not None:
                deps.add(w)
        for b in writes:
            w = self.last_writer.get(b)
            if w is not None:
                deps.add(w)
            for r in self.readers.get(b, ()):
                deps.add(r)
        op.deps = deps
        for b in writes:
            self.last_writer[b] = op
            self.readers[b] = []
        for b in reads:
            self.readers.setdefault(b, []).append(op)
        self.ops[eng].append(op)
        return op

    def barrier(self, fn):
        op = self.add("pool", fn, reads=(), writes=["_bar"])
        return op

    KDMA = 16

    def emit(self, sems, block):
        K = self.KDMA
        plan = {}
        for e in self.ENGS:
            waited = {y: -1 for y in self.ENGS if y != "sp"}
            waited_sp = [-1] * K
            for op in self.ops[e]:
                need = {}
                need_sp = {}
                for d in op.deps:
                    if d is op:
                        continue
                    if d.eng == "sp":
                        j = d.idx % K
                        if d.idx > need_sp.get(j, -1):
                            need_sp[j] = d.idx
                        continue
                    if d.eng == e and e == "pe":
                        continue
                    if d.idx > need.get(d.eng, -1):
                        need[d.eng] = d.idx
                if e == "sp" and op.idx >= K:
                    j = op.idx % K
                    if op.idx - K > need_sp.get(j, -1):
                        need_sp[j] = op.idx - K
                lst = []
                for y, i in need.items():
                    if i > waited[y]:
                        lst.append((y, i))
                        waited[y] = i
                        self.ops[y][i].milestone = True
                for j, i in need_sp.items():
                    if i > waited_sp[j]:
                        lst.append(("sp", i))
                        waited_sp[j] = i
                plan[op] = lst
        for e in self.ENGS:
            if e == "sp":
                for op in self.ops[e]:
                    op.milestone = True
                    op.semval = 16 * (op.idx // K + 1)
                continue
            c = 0
            for op in self.ops[e]:
                if op.milestone:
                    c += 1
                    op.semval = c

        def semof(y, i):
            return sems["sp"][i % K] if y == "sp" else sems[y]

        def run_engine(e, eng):
            for op in self.ops[e]:
                for (y, i) in plan[op]:
                    eng.wait_ge(semof(y, i), self.ops[y][i].semval)
                ins = op.fn(eng)
                if op.milestone:
                    if e == "sp":
                        ins.then_inc(sems["sp"][op.idx % K], 16)
                    else:
                        ins.then_inc(sems[e], 1)
            if e == "sp":
                n = len(self.ops["sp"])
                for i in range(max(0, n - K), n):
                    eng.wait_ge(sems["sp"][i % K], self.ops["sp"][i].semval)

        @block.tensor
        def _(eng):
            run_engine("pe", eng)

        @block.scalar
        def _(eng):
            run_engine("act", eng)

        @block.vector
        def _(eng):
            run_engine("dve", eng)

        @block.gpsimd
        def _(eng):
            run_engine("pool", eng)

        @block.sync
        def _(eng):
            run_engine("sp", eng)


ARENA_KB = 207


def build(dbg=False, upto=None):
    nc = bass.Bass("TRN2", target_bir_lowering=False)

    def din(name, shape):
        return nc.dram_tensor(name, list(shape), F32, kind="ExternalInput").ap()

    x_d = din("x", [S, D])
    p_d = din("p", [S, 256])
    win_d = din("w_in", [D, 4096])
    wpool_d = din("w_pool", [4, 128, 128])
    pscale_d = din("pscale", [128, 4])
    wpu_d = din("w_pu", [512, D])
    wau_d = din("w_au", [512, D])
    wo_d = din("w_o", [D, D])
    ln1g_d = din("ln1_g", [D])
    ln1b_d = din("ln1_b", [D])
    wr_d = din("wr", [D, 36])
    br_d = din("br", [36])
    weg_d = din("w_eg", [NEXP, D, 512])
    weu_d = din("w_eu", [NEXP, D, 512])
    wed_d = din("w_ed", [NEXP, 512, D])
    wpg_d = din("w_pg", [D, D])
    wpp_d = din("w_pp", [256, D])
    ln2g_d = din("ln2_g", [D])
    ln2b_d = din("ln2_b", [D])
    cst_d = din("cst", [128, 448])
    out_d = nc.dram_tensor("out", [S, D], F32, kind="ExternalOutput").ap()
    dbg_outs = {}

    P = Prog()
    with ExitStack() as es:
        arena = es.enter_context(nc.sbuf_tensor("arena", [128, ARENA_KB * 256], F32))
        ps = es.enter_context(nc.psum_tensor("ps", [128, 4096], F32))
        sems = {e: es.enter_context(nc.semaphore("s_" + e)) for e in Prog.ENGS if e != "sp"}
        sems["sp"] = [es.enter_context(nc.semaphore("s_dma%d" % j)) for j in range(Prog.KDMA)]
        block = es.enter_context(nc.Block())

        def R(kb, nkb, dt=F32):
            a = arena[:, int(kb * 256):int((kb + nkb) * 256)]
            return a if dt == F32 else a.bitcast(dt)

        def bank(b, n=1):
            return ps[:, b * 512:(b + n) * 512]

        def cast(eng, out, in_, reads, writes):
            if eng == "act":
                P.add("act", lambda e: e.copy(out=out, in_=in_), reads=reads, writes=writes)
            else:
                P.add(eng, lambda e: e.tensor_copy(out=out, in_=in_), reads=reads, writes=writes)

        def dbg_dump(name, ap, reads):
            if not dbg:
                return
            shp = list(ap.shape)
            d = nc.dram_tensor("dbg_" + name, shp, ap.dtype, kind="ExternalOutput").ap()
            dbg_outs[name] = d
            n = shp[1]
            step = 2048
            for a in range(0, n, step):
                b = min(n, a + step)
                P.add("sp", lambda e, a=a, b=b: e.dma_start(out=d[:, a:b], in_=ap[:, a:b]), reads=reads)

        def phase(name):
            order = ["A", "B", "B1", "B2", "B3", "D", "E", "F", "F1", "F2", "F3", "F3b", "F3c", "F3d", "F4", "F5", "G", "I", "H", "J"]
            if upto is not None and order.index(name) > order.index(upto):
                P.enabled = False

        ident = R(0, 0.5)
        cstf = R(0.5, 1.75)
        cbf = R(2.25, 0.75, BF16).rearrange("p (a n) -> p a n", a=3)
        triU, ones_bf, mask01 = cbf[:, 0, :], cbf[:, 1, :], cbf[:, 2, :]
        rcs = cstf[:, 384:448].rearrange("p (g n) -> p g n", g=4)
        g_bc = R(3, 4)
        b_bc = R(7, 4)
        pscale = R(11, 0.0625)[:, 0:4]
        br_bc = R(11.0625, 0.1875)[:, 0:36]
        wpool_bf = R(11.25, 1, BF16).rearrange("p (g n) -> p g n", g=4)
        XT = R(14, 32, BF16).rearrange("p (k n) -> p k n", k=8)

        zeros_bf = R(12.25, 0.25, BF16)
        P.add("pool", lambda e: e.memset(zeros_bf, 0.0), writes=["zeros_bf"])
        identd = din("ident", [128, 128])
        P.add("sp", lambda e: e.dma_start(out=ident, in_=identd), writes=["ident"])
        P.add("sp", lambda e: e.dma_start(out=cstf, in_=cst_d), writes=["cstf"])
        P.add("pool", lambda e: e.tensor_copy(out=R(2.25, 0.75, BF16), in_=cstf[:, 0:384]), reads=["cstf"], writes=["cbf"])
        P.add("sp", lambda e: e.dma_start(out=pscale, in_=pscale_d), writes=["pscale"])
        P.add("sp", lambda e: e.dma_start(out=g_bc, in_=ln1g_d.partition_broadcast(128)), writes=["g_bc"])
        P.add("sp", lambda e: e.dma_start(out=b_bc, in_=ln1b_d.partition_broadcast(128)), writes=["b_bc"])
        P.add("sp", lambda e: e.dma_start(out=br_bc, in_=br_d.partition_broadcast(128)), writes=["br_bc"])

        PO = R(46, 16, BF16).rearrange("p (k n) -> p k n", k=4)
        AO = R(62, 16, BF16).rearrange("p (k n) -> p k n", k=4)
        QT = R(78, 16, BF16).rearrange("p (k n) -> p k n", k=4)
        NKTp = R(94, 32, BF16).rearrange("p (h n) -> p h n", h=8)
        Vp = R(126, 32, BF16).rearrange("p (i m r d) -> p i m r d", i=16, m=4, r=2)
        wst = [R(158, 16).rearrange("p (k n) -> p k n", k=8), R(174, 16).rearrange("p (k n) -> p k n", k=8)]
        wbf = [R(190, 8, BF16).rearrange("p (k n) -> p k n", k=8), R(198, 8, BF16).rearrange("p (k n) -> p k n", k=8)]

        def xT_keys(tc):
            return [("xT", 4 * tc + j) for j in range(4)]

        def load_win_block(cb, s, eng="pool"):
            P.add("sp", lambda e: e.dma_start(out=wst[s], in_=win_d[:, cb * 512:(cb + 1) * 512].rearrange("(k p) n -> p k n", p=128)),
                  writes=[("wst", s)])
            cast(eng, wbf[s], wst[s], [("wst", s)], [("wbf", s)])

        wpool_st2 = R(70, 2).rearrange("p (g n) -> p g n", g=4)
        P.add("sp", lambda e: e.dma_start(out=wpool_st2, in_=wpool_d.rearrange("g p n -> p g n")), writes=["wpool_st"])
        P.add("pool", lambda e: e.tensor_copy(out=wpool_bf, in_=wpool_st2), reads=["wpool_st"], writes=["wpool_bf"])

        load_win_block(0, 0, "act")
        load_win_block(1, 1, "act")
        xs = [R(62, 4), R(66, 4)]
        for i in range(NT):
            b = i % 2
            P.add("sp", lambda e, i=i, b=b: e.dma_start(out=xs[b], in_=x_d[i * 128:(i + 1) * 128, :]), writes=[("xs", b)])
            for k in range(8):
                P.add("pe", lambda e, k=k, b=b: e.transpose(out=ps[:, b * 1024 + k * 128: b * 1024 + (k + 1) * 128],
                                                            in_=xs[b][:, k * 128:(k + 1) * 128], identity=ident),
                      reads=[("xs", b), "ident"], writes=[("ps", 2 * b + k // 4)])
            P.add("act", lambda e, i=i, b=b: e.copy(out=XT[:, 0:4, i * 128:(i + 1) * 128],
                                                     in_=bank(2 * b).rearrange("p (k n) -> p k n", k=4)),
                  reads=[], writes=[("xT", i), ("ps", 2 * b)])
            P.add("dve", lambda e, i=i, b=b: e.tensor_copy(out=XT[:, 4:8, i * 128:(i + 1) * 128],
                                                            in_=bank(2 * b + 1).rearrange("p (k n) -> p k n", k=4)),
                  reads=[], writes=[("xT", i), ("ps", 2 * b + 1)])

        NKTp4 = R(94, 32, BF16).rearrange("p (m r n) -> p m r n", m=4, r=2)
        P.add("act", lambda e: e.memzero(NKTp4[64:128, :, 0, :]), writes=["NKTp"])
        P.add("act", lambda e: e.memzero(NKTp4[0:64, :, 1, :]), writes=["NKTp"])

        phase("B")
        phase("B1")
        up = R(126, 8)
        tmp = [R(134, 8), R(142, 8)]
        pooled = R(150, 4, BF16)
        pb_rot = [0]

        def next_bank():
            b = pb_rot[0] % 8
            pb_rot[0] += 1
            return b

        evac_rot = [0]

        def evac(out, in_, reads, writes, scale=None):
            evac_rot[0] += 1
            if evac_rot[0] % 2 == 0:
                if scale is None:
                    P.add("act", lambda e: e.copy(out=out, in_=in_), reads=reads, writes=writes)
                else:
                    P.add("act", lambda e: e.mul(out=out, in_=in_, mul=scale), reads=reads, writes=writes)
            else:
                if scale is None:
                    P.add("dve", lambda e: e.tensor_copy(out=out, in_=in_), reads=reads, writes=writes)
                else:
                    P.add("dve", lambda e: e.tensor_scalar(out=out, in0=in_, scalar1=scale, scalar2=None, op0=ALU.mult),
                          reads=reads, writes=writes)

        def proj_fm(wslot, mcols, tc, out_ap, out_key, scale=None):
            b = next_bank()
            for k in range(8):
                P.add("pe", lambda e, k=k, b=b: e.matmul(bank(b), lhsT=wbf[wslot][:, k, mcols[0]:mcols[1]],
                                                         rhs=XT[:, k, tc * 512:(tc + 1) * 512], start=(k == 0), stop=(k == 7)),
                      reads=[("wbf", wslot)] + xT_keys(tc), writes=[("ps", b)])
            evac(out_ap, bank(b), [("ps", b)], [out_key], scale)

        WIN = (2, 4, 8, 16)
        for g in range(4):
            for tc in range(4):
                proj_fm(0, (g * 128, (g + 1) * 128), tc, up[:, tc * 512:(tc + 1) * 512], "up")
            src, srck = up, "up"
            step = 1
            ti = 0
            while step < WIN[g]:
                dst, dstk = tmp[ti % 2], ("tmp", ti % 2)
                P.add("dve", lambda e, src=src, dst=dst, step=step: e.tensor_tensor(out=dst[:, step:], in0=src[:, step:], in1=src[:, :S - step], op=ALU.add),
                      reads=[srck], writes=[dstk])
                P.add("pool", lambda e, src=src, dst=dst, step=step: e.tensor_copy(out=dst[:, 0:step], in_=src[:, 0:step]),
                      reads=[srck], writes=[dstk])
                src, srck = dst, dstk
                step *= 2
                ti += 1
            w = WIN[g]
            P.add("dve", lambda e, src=src, w=w: e.scalar_tensor_tensor(out=pooled[:, 16:], in0=src[:, 16:], scalar=1.0 / w, in1=up[:, 16:],
                                                                         op0=ALU.mult, op1=ALU.subtract),
                  reads=[srck, "up"], writes=["pooled"])
            hd = tmp[ti % 2][:, 0:16]
            hdk = ("tmp", ti % 2)
            P.add("dve", lambda e, src=src, g=g, hd=hd: e.tensor_tensor(out=hd, in0=src[:, 0:16], in1=rcs[:, g, :], op=ALU.mult),
                  reads=[srck, "cstf"], writes=[hdk])
            P.add("dve", lambda e, hd=hd: e.tensor_tensor(out=pooled[:, 0:16], in0=hd, in1=up[:, 0:16], op=ALU.subtract),
                  reads=[hdk, "up"], writes=["pooled"])
            for tc in range(4):
                proj_fm(1, (g * 128, (g + 1) * 128), tc, QT[:, g, tc * 512:(tc + 1) * 512], "QT")
            for tc in range(4):
                b = next_bank()
                P.add("pe", lambda e, g=g, tc=tc, b=b: e.matmul(bank(b), lhsT=wpool_bf[:, g, :], rhs=pooled[:, tc * 512:(tc + 1) * 512], start=True, stop=True),
                      reads=["wpool_bf", "pooled"], writes=[("ps", b)])
                P.add("act", lambda e, g=g, tc=tc, b=b: e.activation(out=PO[:, g, tc * 512:(tc + 1) * 512], in_=bank(b), func=AF.Identity, scale=pscale[:, g:g + 1]),
                      reads=[("ps", b), "pscale"], writes=["PO"])
        dbg_dump("po", R(46, 16, BF16), ["PO"])

        phase("B2")
        load_win_block(2, 0, "act")
        for m in range(4):
            for tc in range(4):
                b = next_bank()
                for k in range(8):
                    P.add("pe", lambda e, k=k, b=b, m=m, tc=tc: e.matmul(bank(b), lhsT=wbf[0][:, k, m * 128:(m + 1) * 128],
                                                                       rhs=XT[:, k, tc * 512:(tc + 1) * 512], start=(k == 0), stop=(k == 7)),
                          reads=[("wbf", 0)] + xT_keys(tc), writes=[("ps", b)])
                P.add("act", lambda e, b=b, m=m, tc=tc: e.mul(out=NKTp[0:64, 2 * m, tc * 512:(tc + 1) * 512], in_=bank(b)[0:64, :], mul=-0.125),
                      reads=[], writes=["NKTp", ("ps", b)])
                P.add("dve", lambda e, b=b, m=m, tc=tc: e.tensor_scalar(out=NKTp[64:128, 2 * m + 1, tc * 512:(tc + 1) * 512], in0=bank(b)[64:128, :],
                                                                     scalar1=-0.125, scalar2=None, op0=ALU.mult),
                      reads=[], writes=["NKTp", ("ps", b)])
        phase("B3")
        P.add("act", lambda e: e.memzero(Vp[:, :, :, 0, 64:128]), writes=["Vp", "up", ("tmp", 0), ("tmp", 1), "pooled"])
        P.add("dve", lambda e: e.memset(Vp[:, :, :, 1, 0:64], 0.0), writes=["Vp", "up", ("tmp", 0), ("tmp", 1), "pooled"])
        load_win_block(3, 1, "act")
        for i in range(NT):
            b = next_bank()
            for k in range(8):
                P.add("pe", lambda e, k=k, b=b, i=i: e.matmul(bank(b), lhsT=XT[:, k, i * 128:(i + 1) * 128], rhs=wbf[1][:, k, :], start=(k == 0), stop=(k == 7)),
                      reads=[("wbf", 1), ("xT", i)], writes=[("ps", b)])
            src5 = bank(b).rearrange("p (m r d) -> p m r d", m=4, r=2)
            P.add("act", lambda e, i=i, src5=src5: e.copy(out=Vp[:, i, :, 0, 0:64], in_=src5[:, :, 0, :]), reads=[], writes=["Vp", ("ps", b)])
            P.add("dve", lambda e, i=i, src5=src5: e.tensor_copy(out=Vp[:, i, :, 1, 64:128], in_=src5[:, :, 1, :]), reads=[], writes=["Vp", ("ps", b)])
        P.barrier(lambda e: e.memset(R(206, 0.25), 0.0))

        phase("D")
        wpu_bf = R(176, 8, BF16).rearrange("p (k n) -> p k n", k=4)
        wau_bf = R(184, 8, BF16).rearrange("p (k n) -> p k n", k=4)
        wpst = R(192, 8).rearrange("p (k n) -> p k n", k=2)
        prefetch_jobs = [(wd_, wb_, wk_, hk) for (wd_, wb_, wk_) in ((wpu_d, wpu_bf, "wpu_bf"), (wau_d, wau_bf, "wau_bf")) for hk in range(2)]

        def prefetch_job(j):
            if j >= 1:
                wd_, wb_, wk_, hk = prefetch_jobs[j - 1]
                P.add("dve", lambda e: e.tensor_copy(out=wb_[:, 2 * hk:2 * hk + 2, :], in_=wpst), reads=["wpst"], writes=[wk_])
            if j < len(prefetch_jobs):
                wd2, wb2, wk2, hk2 = prefetch_jobs[j]
                P.add("sp", lambda e: e.dma_start(out=wpst, in_=wd2[hk2 * 256:(hk2 + 1) * 256, :].rearrange("(k p) n -> p k n", p=128)), writes=["wpst"])
        Eb = [R(158, 4), R(162, 4)]
        SPb = [R(166, 2, BF16), R(168, 2, BF16)]
        Sb = R(170, 2, BF16)
        Ab = [R(172, 2, BF16), R(174, 2, BF16)]
        Zs = ps[:, 0:1024]
        PBs = [ps[:, 1024:2048], ps[:, 2048:3072]]
        Op_ = ps[:, 3072:4096]

        units = []
        for h in range(8):
            for hf in range(2):
                for kb in range(8 * hf + 7, -1, -1):
                    units.append((h, hf, kb))

        def pieces(c0):
            out = []
            if c0 < 512:
                out.append((c0, 512))
                out.append((512, 1024))
            else:
                out.append((c0, 1024))
            return out

        def stage1(u, ui):
            h, hf, kb = u
            m, pb = h // 2, (h % 2) * 64
            zb = ui % 2
            c0 = max(kb * 128 - hf * 1024, 0)
            diag = kb * 128 >= hf * 1024
            for (a, b) in pieces(c0):
                P.add("pe", lambda e, a=a, b=b: e.matmul(Zs[:, a:b], lhsT=NKTp[:, h, kb * 128:(kb + 1) * 128],
                                                         rhs=QT[:, m, hf * 1024 + a: hf * 1024 + b], start=True, stop=True),
                      reads=["NKTp", "QT"], writes=["Z"])
            P.add("act", lambda e: e.activation(out=Eb[zb][:, c0:], in_=Zs[:, c0:], func=AF.Exp, scale=-1.0),
                  reads=["Z"], writes=[("E", zb)])

        def stage1b(u, ui):
            h, hf, kb = u
            zb = ui % 2
            c0 = max(kb * 128 - hf * 1024, 0)
            diag = kb * 128 >= hf * 1024
            P.add("act", lambda e: e.activation(out=SPb[zb][:, c0:], in_=Eb[zb][:, c0:], func=AF.Ln, bias=1.0),
                  reads=[("E", zb)], writes=[("SP", zb)])
            if diag:
                P.add("dve", lambda e: e.tensor_tensor(out=SPb[zb][:, c0:c0 + 128], in0=SPb[zb][:, c0:c0 + 128], in1=mask01, op=ALU.mult),
                      reads=[("SP", zb), "cbf"], writes=[("SP", zb)])

        def stage2(u, ui):
            h, hf, kb = u
            m, pb = h // 2, (h % 2) * 64
            zb = ui % 2
            c0 = max(kb * 128 - hf * 1024, 0)
            diag = kb * 128 >= hf * 1024
            first = (kb == 8 * hf + 7)
            if first:
                P.add("dve", lambda e: e.memset(Sb, 0.0), writes=["S"])
            for (a, b) in pieces(c0):
                P.add("pe", lambda e, a=a, b=b: e.matmul(PBs[zb][:, a:b], lhsT=triU, rhs=SPb[zb][:, a:b], start=True, stop=False),
                      reads=[("SP", zb), "cbf"], writes=[("PB", zb)])
                if not first:
                    P.add("pe", lambda e, a=a, b=b: e.matmul(PBs[zb][:, a:b], lhsT=ones_bf, rhs=Sb[:, a:b], start=False, stop=False),
                          reads=["S", "cbf"], writes=[("PB", zb)])
                P.add("pe", lambda e, a=a, b=b: e.matmul(PBs[zb][:, a:b], lhsT=NKTp[:, h, kb * 128:(kb + 1) * 128],
                                                         rhs=QT[:, m, hf * 1024 + a: hf * 1024 + b], start=False, stop=True),
                      reads=["NKTp", "QT"], writes=[("PB", zb)])
            P.add("act", lambda e: e.activation(out=Ab[zb][:, c0:], in_=PBs[zb][:, c0:], func=AF.Exp, scale=-1.0),
                  reads=[("PB", zb)], writes=[("A", zb)])
            if diag:
                P.add("dve", lambda e: e.tensor_tensor(out=Ab[zb][:, c0:c0 + 128], in0=Ab[zb][:, c0:c0 + 128], in1=mask01, op=ALU.mult),
                      reads=[("A", zb), "cbf"], writes=[("A", zb)])
            if kb > 0:
                P.add("dve", lambda e: e.tensor_tensor(out=Sb[:, c0:], in0=Sb[:, c0:], in1=SPb[zb][:, c0:], op=ALU.add),
                      reads=["S", ("SP", zb)], writes=["S"])

        def stage3(u, ui):
            h, hf, kb = u
            m, pb = h // 2, (h % 2) * 64
            zb = ui % 2
            c0 = max(kb * 128 - hf * 1024, 0)
            if kb == 8 * hf + 7:
                for j in range(2):
                    P.add("pe", lambda e, j=j: e.matmul(Op_[:, j * 512:(j + 1) * 512], lhsT=zeros_bf, rhs=QT[:, 0, 0:512],
                                                        start=True, stop=False, skip_group_check=True),
                          reads=["zeros_bf", "QT"], writes=["O"])
            for (a, b) in pieces(c0):
                P.add("pe", lambda e, a=a, b=b: e.matmul(Op_[:, a:b], lhsT=Vp[:, kb, m, h % 2, :], rhs=Ab[zb][:, a:b],
                                                         start=False, stop=(kb == 0), skip_group_check=True),
                      reads=["Vp", ("A", zb)], writes=["O"])
            if kb == 0:
                P.add("dve", lambda e: e.tensor_copy(out=AO[pb:pb + 64, m, hf * 1024:(hf + 1) * 1024], in_=Op_[pb:pb + 64, :]),
                      reads=["O"], writes=["AO"])

        nu = len(units)
        for it in range(nu + 2):
            if it < nu:
                stage1(units[it], it)
                stage1b(units[it], it)
            if 0 <= it - 1 < nu:
                stage2(units[it - 1], it - 1)
            if 0 <= it - 2 < nu:
                stage3(units[it - 2], it - 2)
            if it % 12 == 11 and it // 12 <= len(prefetch_jobs):
                prefetch_job(it // 12)
        dbg_dump("ao", R(62, 16, BF16), ["AO"])
        P.barrier(lambda e: e.memset(R(206, 0.25), 0.0))

        phase("E")
        MG = R(78, 32, BF16).rearrange("p (k n) -> p k n", k=8)
        gw = [R(110 + 8 * j, 8, BF16).rearrange("p (k n) -> p k n", k=8) for j in range(4)]
        gst = [R(142, 16), R(158, 16)]
        etmp = [[R(192 + 4 * j + 2 * q, 2) for q in range(2)] for j in range(2)]


        def load_gate_block(cb, slot, s):
            P.add("sp", lambda e: e.dma_start(out=gst[s].rearrange("p (k n) -> p k n", k=8),
                                              in_=win_d[:, cb * 512:(cb + 1) * 512].rearrange("(k p) n -> p k n", p=128)), writes=[("gst", s)])
            cast(("act", "dve", "act", "dve")[slot], gw[slot], gst[s].rearrange("p (k n) -> p k n", k=8), [("gst", s)], [("gw", slot)])

        load_gate_block(4, 0, 0)
        load_gate_block(6, 1, 1)
        load_gate_block(5, 2, 0)
        load_gate_block(7, 3, 1)
        for hh in range(2):
            P.add("sp", lambda e, hh=hh: e.dma_start(out=gst[hh].rearrange("p (k n) -> p k n", k=8),
                                                     in_=wo_d[:, hh * 512:(hh + 1) * 512].rearrange("(k p) n -> p k n", p=128)), writes=[("gst", hh)])
        it = 0
        for pair in range(2):
            for cc in range(4):
                c = pair * 4 + cc
                for tc in range(4):
                    pbs = (it % 2) * 4
                    tb = etmp[it % 2]
                    it += 1
                    for (bi, slot) in ((0, 2 * pair), (1, 2 * pair + 1)):
                        for k in range(8):
                            P.add("pe", lambda e, k=k, bi=bi, slot=slot, pbs=pbs, cc=cc, tc=tc: e.matmul(
                                bank(pbs + bi), lhsT=gw[slot][:, k, cc * 128:(cc + 1) * 128], rhs=XT[:, k, tc * 512:(tc + 1) * 512],
                                start=(k == 0), stop=(k == 7)),
                                reads=[("gw", slot)] + xT_keys(tc), writes=[("ps", pbs + bi)])
                    for (bi, wsb, src, key, wkey) in ((2, wpu_bf, PO, "PO", "wpu_bf"), (3, wau_bf, AO, "AO", "wau_bf")):
                        for k in range(4):
                            P.add("pe", lambda e, k=k, bi=bi, wsb=wsb, src=src, pbs=pbs, c=c, tc=tc: e.matmul(
                                bank(pbs + bi), lhsT=wsb[:, k, c * 128:(c + 1) * 128], rhs=src[:, k, tc * 512:(tc + 1) * 512],
                                start=(k == 0), stop=(k == 3)),
                                reads=[key, wkey], writes=[("ps", pbs + bi)])
                    P.add("act", lambda e, pbs=pbs, tb=tb: e.activation(out=tb[0], in_=bank(pbs + 0), func=AF.Sigmoid),
                          reads=[("ps", pbs + 0)], writes=[("et", it % 2, 0)])
                    P.add("act", lambda e, pbs=pbs, tb=tb: e.activation(out=tb[1], in_=bank(pbs + 1), func=AF.Sigmoid),
                          reads=[("ps", pbs + 1)], writes=[("et", it % 2, 1)])
                    P.add("dve", lambda e, pbs=pbs, tb=tb: e.tensor_tensor(out=tb[0], in0=bank(pbs + 2), in1=tb[0], op=ALU.mult),
                          reads=[("ps", pbs + 2), ("et", it % 2, 0)], writes=[("et", it % 2, 0)])
                    P.add("dve", lambda e, pbs=pbs, tb=tb: e.tensor_tensor(out=tb[1], in0=bank(pbs + 3), in1=tb[1], op=ALU.mult),
                          reads=[("ps", pbs + 3), ("et", it % 2, 1)], writes=[("et", it % 2, 1)])
                    P.add("pool", lambda e, tb=tb, c=c, tc=tc: e.tensor_tensor(out=MG[:, c, tc * 512:(tc + 1) * 512], in0=tb[0], in1=tb[1], op=ALU.add),
                          reads=[("et", it % 2, 0), ("et", it % 2, 1)], writes=["MG"])
        dbg_dump("mg", R(78, 32, BF16), ["MG"])
        P.barrier(lambda e: e.memset(R(206, 0.25), 0.0))

        phase("F")
        ACC = R(110, 64).rearrange("p (t n) -> p t n", t=NT)
        wo_bf = R(174, 16, BF16).rearrange("p (k n) -> p k n", k=8)
        wo_st = R(190, 16).rearrange("p (k n) -> p k n", k=8)
        xr = [R(46, 4), R(50, 4)]
        rbufs = [R(54, 4), R(190, 4)]
        x1bs = [R(58, 4), R(194, 4)]
        sqb = R(62, 2, BF16)
        x1Tfs = [R(64, 4).rearrange("p (k n) -> p k n", k=8), R(198, 4).rearrange("p (k n) -> p k n", k=8)]
        LOG = R(68, 2.25).rearrange("p (t n) -> p t n", t=NT)
        wr_sb = R(70.5, 1.125).rearrange("p (k n) -> p k n", k=8)
        CMB = R(76, 2).rearrange("p (t n) -> p t n", t=NT)
        STT = R(205.5, 0.5).rearrange("p (t n) -> p t n", t=NT)
        RTS = R(62, 3.5)

        for hh in range(2):
            cast(("act", "dve")[hh], wo_bf[:, :, hh * 512:(hh + 1) * 512], gst[hh].rearrange("p (k n) -> p k n", k=8), [("gst", hh)], [("wo_bf", hh)])
        P.add("sp", lambda e: e.dma_start(out=wr_sb, in_=wr_d.rearrange("(k p) n -> p k n", p=128)), writes=["wr_sb"])

        def ln_stats(i, src, srck, stats, sq, sqk):
            P.add("act", lambda e: e.activation(out=sq, in_=src, func=AF.Square, accum_out=stats[:, 1:2]),
                  reads=[srck], writes=[sqk, ("st", i, 1)])
            P.add("dve", lambda e: e.tensor_scalar(out=stats[:, 2:3], in0=stats[:, 0:1], scalar1=1.0 / D, scalar2=None, op0=ALU.mult),
                  reads=[("st", i, 0)], writes=[("st", i, 2)])
            P.add("dve", lambda e: e.tensor_tensor(out=stats[:, 3:4], in0=stats[:, 2:3], in1=stats[:, 2:3], op=ALU.mult),
                  reads=[("st", i, 2)], writes=[("st", i, 3)])
            P.add("dve", lambda e: e.scalar_tensor_tensor(out=stats[:, 4:5], in0=stats[:, 1:2], scalar=1.0 / D, in1=stats[:, 3:4], op0=ALU.mult, op1=ALU.subtract),
                  reads=[("st", i, 1), ("st", i, 3)], writes=[("st", i, 4)])
            P.add("act", lambda e: e.activation(out=stats[:, 5:6], in_=stats[:, 4:5], func=AF.Ln, bias=EPS),
                  reads=[("st", i, 4)], writes=[("st", i, 5)])
            P.add("act", lambda e: e.activation(out=stats[:, 6:7], in_=stats[:, 5:6], func=AF.Exp, scale=-0.5),
                  reads=[("st", i, 5)], writes=[("st", i, 6)])
            P.add("dve", lambda e: e.scalar_tensor_tensor(out=stats[:, 7:8], in0=stats[:, 2:3], scalar=-1.0, in1=stats[:, 6:7], op0=ALU.mult, op1=ALU.mult),
                  reads=[("st", i, 2), ("st", i, 6)], writes=[("st", i, 7)])

        def f_vars(i):
            b = i % 2
            return (b, ps[:, b * 1024:(b + 1) * 1024], rbufs[b], x1bs[b], x1Tfs[b], ("rbuf", b), ("x1b", b), ("x1Tf", b),
                    [], STT[:, i, :])

        def f_stage_a(i):
            b, yb, rbuf, x1b, x1Tf, rk, xk, tk, alias, st = f_vars(i)
            P.add("sp", lambda e: e.dma_start(out=xr[b], in_=x_d[i * 128:(i + 1) * 128, :]), writes=[("xr", b)])
            for hh in range(2):
                for k in range(8):
                    P.add("pe", lambda e, hh=hh, k=k: e.matmul(yb[:, hh * 512:(hh + 1) * 512], lhsT=MG[:, k, i * 128:(i + 1) * 128],
                                                             rhs=wo_bf[:, k, hh * 512:(hh + 1) * 512], start=(k == 0), stop=(k == 7)),
                          reads=["MG", ("wo_bf", hh)], writes=[("Y", b)])
            P.add("dve", lambda e: e.scalar_tensor_tensor(out=rbuf, in0=xr[b], scalar=ALPHA, in1=yb, op0=ALU.mult, op1=ALU.add, accum_out=st[:, 0:1]),
                  reads=[("xr", b)], writes=[rk, ("st", i, 0), ("Y", b)] + alias)

        def f_stage_b(i):
            b, yb, rbuf, x1b, x1Tf, rk, xk, tk, alias, st = f_vars(i)
            ln_stats(i, rbuf, rk, st, sqb, "sqb")
            P.add("act", lambda e: e.activation(out=x1b, in_=rbuf, func=AF.Identity, scale=st[:, 6:7], bias=st[:, 7:8]),
                  reads=[rk, ("st", i, 6), ("st", i, 7)], writes=[xk] + alias)
            P.add("dve", lambda e: e.tensor_tensor(out=x1b, in0=x1b, in1=g_bc, op=ALU.mult), reads=[xk, "g_bc"], writes=[xk])
            P.add("dve", lambda e: e.tensor_tensor(out=x1b, in0=x1b, in1=b_bc, op=ALU.add), reads=[xk, "b_bc"], writes=[xk])

        def f_stage_c(i):
            b, yb, rbuf, x1b, x1Tf, rk, xk, tk, alias, st = f_vars(i)
            P.add("act", lambda e: e.mul(out=ACC[:, i, :], in_=x1b, mul=ALPHA), reads=[xk], writes=[("ACC", i)])
            for k in range(8):
                P.add("pe", lambda e, k=k: e.transpose(out=ps[:, 2048 + k * 128: 2048 + (k + 1) * 128], in_=x1b[:, k * 128:(k + 1) * 128], identity=ident),
                      reads=[xk, "ident"], writes=[("psT", k // 4)])
            for hh in range(2):
                src3 = bank(4 + hh).rearrange("p (k n) -> p k n", k=4)
                P.add("act", lambda e, hh=hh, src3=src3: e.copy(out=XT[:, 4 * hh:4 * hh + 4, i * 128:(i + 1) * 128], in_=src3),
                      reads=[], writes=[("xT", i), ("psT", hh)])
                P.add("dve", lambda e, hh=hh, src3=src3: e.tensor_copy(out=x1Tf[:, 4 * hh:4 * hh + 4, :], in_=src3),
                      reads=[], writes=[tk, ("psT", hh)] + alias)
            for k in range(8):
                P.add("pe", lambda e, k=k: e.matmul(ps[:, 3072:3108], lhsT=x1Tf[:, k, :], rhs=wr_sb[:, k, :], start=(k == 0), stop=(k == 7)),
                      reads=[tk, "wr_sb"], writes=["psL"])

        def f_stage_d(i):
            P.add("dve", lambda e: e.tensor_tensor(out=LOG[:, i, :], in0=ps[:, 3072:3108], in1=br_bc, op=ALU.add),
                  reads=["br_bc"], writes=["LOG", "psL"])

        psts = [R(78, 16), R(94, 16)]
        for n in range(NT + 2):
            if n < NT:
                f_stage_a(n)
            if n == NT - 1:
                for hh in range(2):
                    P.add("sp", lambda e, hh=hh: e.dma_start(out=psts[hh].rearrange("p (k n) -> p k n", k=8),
                                                             in_=wpg_d[:, hh * 512:(hh + 1) * 512].rearrange("(k p) n -> p k n", p=128)),
                          writes=[("pst", hh), "MG"])
            if 0 <= n - 2 < NT:
                f_stage_c(n - 2)
            if 0 <= n - 1 < NT:
                f_stage_b(n - 1)
            if 0 <= n - 2 < NT:
                f_stage_d(n - 2)
        P.add("sp", lambda e: e.dma_start(out=g_bc, in_=ln2g_d.partition_broadcast(128)), writes=["g_bc"])
        P.add("sp", lambda e: e.dma_start(out=b_bc, in_=ln2b_d.partition_broadcast(128)), writes=["b_bc"])
        if dbg:
            dbg_dump("acc1", R(110, 64), [("ACC", i) for i in range(NT)])
            dbg_dump("log", R(68, 2.25), ["LOG"])

        phase("G")
        def rt(off, n, shape3=None):
            a = RTS[:, off:off + n]
            return a
        T = NT
        gl = LOG[:, :, 0:4]
        el = LOG[:, :, 4:36].rearrange("p t (g e) -> p t g e", g=4)
        gm = RTS[:, 0:16]
        gsum = RTS[:, 16:32]
        gp = RTS[:, 32:48]
        m1 = RTS[:, 48:64]
        m2 = RTS[:, 64:80]
        dd = RTS[:, 80:96]
        w1g = RTS[:, 96:112]
        w2g = RTS[:, 112:128]
        gsub = RTS[:, 128:192].rearrange("p (t n) -> p t n", t=T)
        gmask = RTS[:, 192:256].rearrange("p (t n) -> p t n", t=T)
        sel = RTS[:, 256:384].rearrange("p (t n) -> p t n", t=T)
        sel2 = RTS[:, 384:512].rearrange("p (t n) -> p t n", t=T)
        mk1 = RTS[:, 512:640].rearrange("p (t n) -> p t n", t=T)
        mk2 = RTS[:, 640:768].rearrange("p (t n) -> p t n", t=T)
        tmp8 = RTS[:, 768:896].rearrange("p (t n) -> p t n", t=T)

        def bc(a2, n):
            return a2.unsqueeze(2).to_broadcast([128, T, n])

        router_ops = []

        def dv(fn, reads, writes, eng="dve"):
            router_ops.append((eng, fn, reads, writes))
        dv(lambda e: e.reduce_max(out=gm, in_=gl, axis=AX.X), ["LOG"], ["gm"])
        dv(lambda e: e.tensor_tensor(out=gsub, in0=gl, in1=bc(gm, 4), op=ALU.subtract), ["LOG", "gm"], ["gsub"])
        router_ops.append(("act", lambda e: e.activation(out=gsub, in_=gsub, func=AF.Exp), ["gsub"], ["gsub"]))
        dv(lambda e: e.reduce_sum(out=gsum, in_=gsub, axis=AX.X), ["gsub"], ["gsum"])
        dv(lambda e: e.reciprocal(out=gp, in_=gsum), ["gsum"], ["gp"], eng="dve")
        dv(lambda e: e.tensor_tensor(out=gmask, in0=gl, in1=bc(gm, 4), op=ALU.is_equal), ["LOG", "gm"], ["gmask"])
        dv(lambda e: e.tensor_tensor(out=sel, in0=el[:, :, 0, :], in1=gmask[:, :, 0:1].to_broadcast([128, T, 8]), op=ALU.mult), ["LOG", "gmask"], ["sel"])
        for g in range(1, 4):
            dv(lambda e, g=g: e.tensor_tensor(out=tmp8, in0=el[:, :, g, :], in1=gmask[:, :, g:g + 1].to_broadcast([128, T, 8]), op=ALU.mult), ["LOG", "gmask"], ["tmp8"])
            dv(lambda e: e.tensor_tensor(out=sel, in0=sel, in1=tmp8, op=ALU.add), ["sel", "tmp8"], ["sel"])
        dv(lambda e: e.reduce_max(out=m1, in_=sel, axis=AX.X), ["sel"], ["m1"])
        dv(lambda e: e.tensor_tensor(out=mk1, in0=sel, in1=bc(m1, 8), op=ALU.is_equal), ["sel", "m1"], ["mk1"])
        dv(lambda e: e.scalar_tensor_tensor(out=sel2, in0=mk1, scalar=-1e30, in1=sel, op0=ALU.mult, op1=ALU.add), ["sel", "mk1"], ["sel2"])
        dv(lambda e: e.reduce_max(out=m2, in_=sel2, axis=AX.X), ["sel2"], ["m2"])
        dv(lambda e: e.tensor_tensor(out=mk2, in0=sel2, in1=bc(m2, 8), op=ALU.is_equal), ["sel2", "m2"], ["mk2"])
        dv(lambda e: e.tensor_tensor(out=dd, in0=m2, in1=m1, op=ALU.subtract), ["m1", "m2"], ["dd"])
        router_ops.append(("act", lambda e: e.activation(out=dd, in_=dd, func=AF.Exp), ["dd"], ["dd"]))
        dv(lambda e: e.tensor_scalar(out=dd, in0=dd, scalar1=1.0, scalar2=None, op0=ALU.add), ["dd"], ["dd"])
        dv(lambda e: e.reciprocal(out=w1g, in_=dd), ["dd"], ["w1g"], eng="dve")
        dv(lambda e: e.tensor_tensor(out=w1g, in0=w1g, in1=gp, op=ALU.mult), ["w1g", "gp"], ["w1g"])
        dv(lambda e: e.tensor_tensor(out=w2g, in0=gp, in1=w1g, op=ALU.subtract), ["w1g", "gp"], ["w2g"])
        dv(lambda e: e.tensor_tensor(out=sel, in0=mk1, in1=bc(w1g, 8), op=ALU.mult), ["mk1", "w1g", "sel2"], ["sel"])
        dv(lambda e: e.tensor_tensor(out=tmp8, in0=mk2, in1=bc(w2g, 8), op=ALU.mult), ["mk2", "w2g"], ["tmp8"])
        dv(lambda e: e.tensor_tensor(out=sel, in0=sel, in1=tmp8, op=ALU.add), ["sel", "tmp8"], ["sel"])
        for g in range(4):
            dv(lambda e, g=g: e.tensor_tensor(out=CMB[:, :, 8 * g:8 * g + 8], in0=sel, in1=gmask[:, :, g:g + 1].to_broadcast([128, T, 8]), op=ALU.mult),
               ["sel", "gmask"], ["CMB"])
        P.barrier(lambda e: e.memset(R(206, 0.25), 0.0))
        phase("I")
        wpg_bf = R(174, 16, BF16).rearrange("p (k n) -> p k n", k=8)
        wpp_bf = R(102, 4, BF16).rearrange("p (k n) -> p k n", k=2)
        wppst = R(106, 4).rearrange("p (k n) -> p k n", k=2)
        ptile = [R(54, 1), R(55, 1)]
        pT = [R(56, 0.5, BF16).rearrange("p (k n) -> p k n", k=2), R(56.5, 0.5, BF16).rearrange("p (k n) -> p k n", k=2)]
        sgb = [R(57, 2), R(59, 2)]
        for hh in range(2):
            cast(("act", "dve")[hh], wpg_bf[:, :, hh * 512:(hh + 1) * 512], psts[hh].rearrange("p (k n) -> p k n", k=8), [("pst", hh)], [("wpg_bf", hh)])
        for hc in range(2):
            P.add("sp", lambda e, hc=hc: e.dma_start(out=wppst, in_=wpp_d[:, hc * 512:(hc + 1) * 512].rearrange("(k p) n -> p k n", p=128)),
                  writes=["wppst", ("pst", 1)])
            P.add("pool", lambda e, hc=hc: e.tensor_copy(out=wpp_bf[:, :, hc * 512:(hc + 1) * 512], in_=wppst), reads=["wppst"], writes=["wpp_bf"])

        phase("H")
        slots = [R(78 + 8 * j, 8, BF16) for j in range(4)] + [R(174, 8, BF16), R(182, 8, BF16)]
        stg = [R(190, 8), R(198, 8), R(46, 8)]
        hT = [R(54, 4, BF16).rearrange("p (k n) -> p k n", k=4), R(58, 4, BF16).rearrange("p (k n) -> p k n", k=4)]
        sgx = [R(62, 2), R(64, 2)]
        stg_rot = [0]

        def wviews(e_):
            s0 = (e_ % 2) * 3
            wg = slots[s0].rearrange("p (k n) -> p k n", k=8)
            wu = slots[s0 + 1].rearrange("p (k n) -> p k n", k=8)
            wd = slots[s0 + 2].rearrange("p (k n) -> p k n", k=4)
            return wg, wu, wd, s0

        def load_expert(e_):
            wg, wu, wd, s0 = wviews(e_)
            jobs = []
            for (dsrc, dstv, sk) in ((weg_d, wg, s0), (weu_d, wu, s0 + 1)):
                for hk in range(2):
                    jobs.append((dsrc[e_, hk * 512:(hk + 1) * 512, :].rearrange("(k p) n -> p k n", p=128), dstv[:, 4 * hk:4 * hk + 4, :], sk))
            for hc in range(2):
                jobs.append((wed_d[e_, :, hc * 512:(hc + 1) * 512].rearrange("(k p) n -> p k n", p=128), wd[:, :, hc * 512:(hc + 1) * 512], s0 + 2))
            for (src, dst, sk) in jobs:
                s = stg_rot[0] % 3
                stg_rot[0] += 1
                sv = stg[s].rearrange("p (k n) -> p k n", k=4)
                P.add("sp", lambda e, src=src, sv=sv: e.dma_start(out=sv, in_=src), writes=[("stg", s)])
                alias = []
                if e_ == 0:
                    alias = [("pst", 0), ("pst", 1)]
                elif e_ == 1:
                    alias = ["wpp_bf", "wppst"] if sk == 3 else [("wpg_bf", 0), ("wpg_bf", 1)]
                cast("act" if e_ == 0 else "pool", dst, sv, [("stg", s)], [("slot", sk)] + alias)

        GB = [0, 1]
        UB = [2, 3]
        DB = [4, 5, 6, 7]
        gu_rot = [0]
        d_rot = [0]

        def gu_step(e_, tc):
            wg, wu, wd, s0 = wviews(e_)
            hb = tc % 2
            for dc in range(4):
                q = gu_rot[0] % 2
                gu_rot[0] += 1
                for (bk, wv, sk) in ((GB[q], wg, s0), (UB[q], wu, s0 + 1)):
                    for k in range(8):
                        P.add("pe", lambda e, k=k, bk=bk, wv=wv, dc=dc, tc=tc: e.matmul(bank(bk), lhsT=wv[:, k, dc * 128:(dc + 1) * 128], rhs=XT[:, k, tc * 512:(tc + 1) * 512],
                                                                                   start=(k == 0), stop=(k == 7)),
                              reads=[("slot", sk)] + xT_keys(tc), writes=[("ps", bk)])
                ax = ["CMB"] if (e_ == 0 and tc == 0 and dc < 2) else []
                ah = ([("ptile", 0), ("ptile", 1), ("pT", 0), ("pT", 1), ("sgb", 0), ("sgb", 1)] if (e_ == 0 and tc < 2 and dc == 0) else [])
                P.add("act", lambda e, q=q: e.activation(out=sgx[q], in_=bank(GB[q]), func=AF.Silu), reads=[("ps", GB[q])], writes=[("sgx", q)] + ax)
                P.add("dve", lambda e, q=q, hb=hb, dc=dc: e.tensor_tensor(out=hT[hb][:, dc, :], in0=bank(UB[q]), in1=sgx[q], op=ALU.mult),
                      reads=[("ps", UB[q]), ("sgx", q)], writes=[("hT", hb)] + ah)

        def down_step(e_, tc):
            wg, wu, wd, s0 = wviews(e_)
            hb = tc % 2
            for ti in range(4):
                i = tc * 4 + ti
                for hh in range(2):
                    bk = DB[d_rot[0] % 4]
                    d_rot[0] += 1
                    for dc in range(4):
                        P.add("pe", lambda e, dc=dc, bk=bk, ti=ti, hh=hh, hb=hb, wd=wd: e.matmul(bank(bk), lhsT=hT[hb][:, dc, ti * 128:(ti + 1) * 128],
                                                                                           rhs=wd[:, dc, hh * 512:(hh + 1) * 512], start=(dc == 0), stop=(dc == 3)),
                              reads=[("hT", hb), ("slot", s0 + 2)], writes=[("ps", bk)])
                    P.add("dve", lambda e, bk=bk, i=i, hh=hh, e_=e_: e.scalar_tensor_tensor(out=ACC[:, i, hh * 512:(hh + 1) * 512], in0=bank(bk), scalar=CMB[:, i, e_:e_ + 1],
                                                                                          in1=ACC[:, i, hh * 512:(hh + 1) * 512], op0=ALU.mult, op1=ALU.add),
                          reads=[("ps", bk), "CMB", ("ACC", i)], writes=[("ACC", i)])

        ob = [R(66, 4), R(70, 4)]
        sqjunk = R(0.5, 2, BF16)
        STT2 = R(74, 0.5).rearrange("p (t n) -> p t n", t=NT)
        def j_s1(i):
            b = i % 2
            st = STT2[:, i, :]
            acc_i = ACC[:, i, :]
            P.add("act", lambda e: e.activation(out=sqjunk, in_=acc_i, func=AF.Identity, accum_out=st[:, 0:1]),
                  reads=[("ACC", i)], writes=["sqjunk", ("st2", i, 0)])
            P.add("act", lambda e: e.activation(out=sqjunk, in_=acc_i, func=AF.Square, accum_out=st[:, 1:2]),
                  reads=[("ACC", i)], writes=["sqjunk", ("st2", i, 1)])
            P.add("pool", lambda e: e.tensor_scalar(out=st[:, 2:3], in0=st[:, 0:1], scalar1=1.0 / D, scalar2=None, op0=ALU.mult),
                  reads=[("st2", i, 0)], writes=[("st2", i, 2)])
            P.add("pool", lambda e: e.tensor_tensor(out=st[:, 3:4], in0=st[:, 2:3], in1=st[:, 2:3], op=ALU.mult),
                  reads=[("st2", i, 2)], writes=[("st2", i, 3)])
            P.add("pool", lambda e: e.tensor_scalar(out=st[:, 4:5], in0=st[:, 1:2], scalar1=1.0 / D, scalar2=None, op0=ALU.mult),
                  reads=[("st2", i, 1)], writes=[("st2", i, 4)])
            P.add("pool", lambda e: e.tensor_tensor(out=st[:, 4:5], in0=st[:, 4:5], in1=st[:, 3:4], op=ALU.subtract),
                  reads=[("st2", i, 4), ("st2", i, 3)], writes=[("st2", i, 4)])
            P.add("act", lambda e: e.activation(out=st[:, 5:6], in_=st[:, 4:5], func=AF.Ln, bias=EPS),
                  reads=[("st2", i, 4)], writes=[("st2", i, 5)])
            P.add("act", lambda e: e.activation(out=st[:, 6:7], in_=st[:, 5:6], func=AF.Exp, scale=-0.5),
                  reads=[("st2", i, 5)], writes=[("st2", i, 6)])
            P.add("pool", lambda e: e.tensor_tensor(out=st[:, 7:8], in0=st[:, 2:3], in1=st[:, 6:7], op=ALU.mult),
                  reads=[("st2", i, 2), ("st2", i, 6)], writes=[("st2", i, 7)])
            P.add("pool", lambda e: e.tensor_scalar(out=st[:, 7:8], in0=st[:, 7:8], scalar1=-1.0, scalar2=None, op0=ALU.mult),
                  reads=[("st2", i, 7)], writes=[("st2", i, 7)])

        def j_s2(i):
            b = i % 2
            st = STT2[:, i, :]
            acc_i = ACC[:, i, :]
            P.add("act", lambda e: e.activation(out=ob[b], in_=acc_i, func=AF.Identity, scale=st[:, 6:7], bias=st[:, 7:8]),
                  reads=[("ACC", i), ("st2", i, 6), ("st2", i, 7)], writes=[("ob", b)])
            P.add("dve", lambda e: e.tensor_tensor(out=ob[b], in0=ob[b], in1=g_bc, op=ALU.mult), reads=[("ob", b), "g_bc"], writes=[("ob", b)])
            P.add("dve", lambda e: e.tensor_tensor(out=ob[b], in0=ob[b], in1=b_bc, op=ALU.add), reads=[("ob", b), "b_bc"], writes=[("ob", b)])
            P.add("sp", lambda e: e.dma_start(out=out_d[i * 128:(i + 1) * 128, :], in_=ob[b]), reads=[("ob", b)])


        ln2_queue = []

        def ln2_pump(final=False):
            while ln2_queue:
                k = ln2_queue.pop(0)
                j_s1(k)
                if k >= 1:
                    j_s2(k - 1)
            if final:
                j_s2(NT - 1)

        steps = [(e_, tc) for e_ in range(NEXP) for tc in range(4)]
        load_expert(0)
        it = 0
        for i in range(NT):
            b = i % 2
            P.add("sp", lambda e, i=i, b=b: e.dma_start(out=ptile[b][:, 0:256], in_=p_d[i * 128:(i + 1) * 128, :]), writes=[("ptile", b)])
            for k in range(2):
                P.add("pe", lambda e, k=k, b=b: e.transpose(out=ps[:, 3072 + b * 512 + k * 128: 3072 + b * 512 + (k + 1) * 128], in_=ptile[b][:, k * 128:(k + 1) * 128], identity=ident),
                      reads=[("ptile", b), "ident"], writes=[("ps", 6 + b)])
            P.add("act", lambda e, b=b: e.copy(out=pT[b], in_=ps[:, 3072 + b * 512: 3072 + b * 512 + 256].rearrange("p (k n) -> p k n", k=2)),
                  reads=[], writes=[("pT", b), ("ps", 6 + b)])
            for hh in range(2):
                q = it % 2
                it += 1
                bg, bp = q * 2, q * 2 + 1
                for k in range(8):
                    P.add("pe", lambda e, k=k, i=i, hh=hh, bg=bg: e.matmul(bank(bg), lhsT=XT[:, k, i * 128:(i + 1) * 128], rhs=wpg_bf[:, k, hh * 512:(hh + 1) * 512], start=(k == 0), stop=(k == 7)),
                          reads=[("xT", i), ("wpg_bf", hh)], writes=[("ps", bg)])
                for k in range(2):
                    P.add("pe", lambda e, k=k, b=b, hh=hh, bp=bp: e.matmul(bank(bp), lhsT=pT[b][:, k, :], rhs=wpp_bf[:, k, hh * 512:(hh + 1) * 512], start=(k == 0), stop=(k == 1)),
                          reads=[("pT", b), "wpp_bf"], writes=[("ps", bp)])
                P.add("act", lambda e, q=q, bg=bg: e.activation(out=sgb[q], in_=bank(bg), func=AF.Sigmoid), reads=[("ps", bg)], writes=[("sgb", q)])
                P.add("dve", lambda e, q=q, bp=bp: e.tensor_tensor(out=sgb[q], in0=bank(bp), in1=sgb[q], op=ALU.mult), reads=[("ps", bp), ("sgb", q)], writes=[("sgb", q)])
                P.add("pool", lambda e, q=q, i=i, hh=hh: e.tensor_tensor(out=ACC[:, i, hh * 512:(hh + 1) * 512], in0=ACC[:, i, hh * 512:(hh + 1) * 512], in1=sgb[q], op=ALU.add),
                      reads=[("sgb", q), ("ACC", i)], writes=[("ACC", i)])
            for _ in range(3):
                if router_ops:
                    reng, rfn, rreads, rwrites = router_ops.pop(0)
                    P.add(reng, rfn, reads=rreads, writes=rwrites)
        while router_ops:
            reng, rfn, rreads, rwrites = router_ops.pop(0)
            P.add(reng, rfn, reads=rreads, writes=rwrites)
        if dbg:
            dbg_dump("cmb", R(76, 2), ["CMB"])
        if dbg:
            dbg_dump("acc2", R(110, 64), [("ACC", i) for i in range(NT)])
        for si in range(len(steps) + 1):
            if si < len(steps):
                gu_step(*steps[si])
            if si >= 1:
                down_step(*steps[si - 1])
                pe_, ptc = steps[si - 1]
                if pe_ == NEXP - 1:
                    ln2_queue.extend(range(4 * ptc, 4 * ptc + 4))
                    ln2_pump()
            if si < len(steps):
                e_, tc = steps[si]
                if tc == 0 and e_ + 1 < NEXP:
                    load_expert(e_ + 1)
        phase("J")
        ln2_pump(final=True)

        P.emit(sems, block)
    return nc, dbg_outs


def make_consts():
    j = np.arange(128)
    triU = (j[:, None] >= j[None, :]).astype(np.float32)
    ones = np.ones((128, 128), np.float32)
    mask01 = (j[:, None] < j[None, :]).astype(np.float32)
    rcs = np.zeros((4, 16), np.float32)
    for g, w in enumerate((2, 4, 8, 16)):
        rcs[g] = 1.0 / np.minimum(np.arange(1, 17), w)
    rcs = np.broadcast_to(rcs.reshape(1, 64), (128, 64))
    return np.ascontiguousarray(np.concatenate([triU, ones, mask01, rcs], axis=1).astype(np.float32))


def prep_shared(w_in, w_pool, pool_scale, w_pu, w_au, w_o, ln1_g, ln1_b, w_rg, b_rg, w_re, b_re,
                w_eg, w_eu, w_ed, w_pg, w_pp, ln2_g, ln2_b):
    f = lambda a: np.ascontiguousarray(np.asarray(a, dtype=np.float32))
    wr = np.concatenate([np.asarray(w_rg[0]), np.transpose(np.asarray(w_re[0]), (1, 0, 2)).reshape(D, 32)], axis=1)
    br = np.concatenate([np.asarray(b_rg[0]), np.asarray(b_re[0]).reshape(32)])
    return {
        "w_in": f(w_in[0]), "w_pool": f(w_pool[0]), "pscale": f(np.asarray(pool_scale[0]).reshape(4, 128).T),
        "w_pu": f(w_pu[0]), "w_au": f(w_au[0]), "w_o": f(w_o[0]), "ln1_g": f(ln1_g[0]), "ln1_b": f(ln1_b[0]),
        "wr": f(wr), "br": f(br),
        "w_eg": f(np.asarray(w_eg[0]).reshape(NEXP, D, 512)), "w_eu": f(np.asarray(w_eu[0]).reshape(NEXP, D, 512)),
        "w_ed": f(np.asarray(w_ed[0]).reshape(NEXP, 512, D)),
        "w_pg": f(w_pg[0]), "w_pp": f(w_pp[0]), "ln2_g": f(ln2_g[0]), "ln2_b": f(ln2_b[0]),
        "cst": make_consts(), "ident": np.eye(128, dtype=np.float32),
    }


def kernel(x, p, w_in, w_pool, pool_scale, w_pu, w_au, w_o, ln1_g, ln1_b,
           w_rg, b_rg, w_re, b_re, w_eg, w_eu, w_ed, w_pg, w_pp, ln2_g, ln2_b):
    x = np.asarray(x, dtype=np.float32)
    p = np.asarray(p, dtype=np.float32)
    shared = prep_shared(w_in, w_pool, pool_scale, w_pu, w_au, w_o, ln1_g, ln1_b, w_rg, b_rg, w_re, b_re,
                         w_eg, w_eu, w_ed, w_pg, w_pp, ln2_g, ln2_b)
    n = x.shape[0]
    nc, _ = build(False)
    in_maps = []
    for c in range(n):
        m = dict(shared)
        m["x"] = np.ascontiguousarray(x[c])
        m["p"] = np.ascontiguousarray(p[0, c])
        in_maps.append(m)
    res = run_bass_kernel_spmd(nc, in_maps, core_ids=list(range(n)))
    return np.stack([np.asarray(r["out"], dtype=np.float32) for r in res.results], axis=0)
```

```python
import numpy as np
from contextlib import ExitStack
import concourse.bass as bass
import concourse.mybir as mybir
from concourse.bass_utils import run_bass_kernel_spmd

F32 = mybir.dt.float32
BF16 = mybir.dt.bfloat16
AF = mybir.ActivationFunctionType
ALU = mybir.AluOpType
AX = mybir.AxisListType

S = 2048
D = 1024
NT = S // 128
ALPHA = 2.0 ** 0.25
EPS = 1e-5
NEXP = 32


class Op:
    __slots__ = ("eng", "fn", "idx", "deps", "milestone", "semval")


class Prog:
    ENGS = ("pe", "act", "dve", "pool", "sp")

    def __init__(self):
        self.ops = {e: [] for e in self.ENGS}
        self.last_writer = {}
        self.readers = {}
        self.enabled = True

    def add(self, eng, fn, reads=(), writes=()):
        if not self.enabled:
            return None
        op = Op()
        op.eng = eng
        op.fn = fn
        op.idx = len(self.ops[eng])
        op.milestone = False
        op.semval = None
        deps = set()
        reads = list(reads) + ["_bar"]
        for b in reads:
            w = self.last_writer.get(b)
            if w is not None:
                deps.add(w)
        for b in writes:
            w = self.last_writer.get(b)
            if w is not None:
                deps.add(w)
            for r in self.readers.get(b, ()):
                deps.add(r)
        op.deps = deps
        for b in writes:
            self.last_writer[b] = op
            self.readers[b] = []
        for b in reads:
            self.readers.setdefault(b, []).append(op)
        self.ops[eng].append(op)
        return op

    def barrier(self, fn):
        op = self.add("pool", fn, reads=(), writes=["_bar"])
        return op

    KDMA = 16

    def emit(self, sems, block):
        K = self.KDMA
        plan = {}
        for e in self.ENGS:
            waited = {y: -1 for y in self.ENGS if y != "sp"}
            waited_sp = [-1] * K
            for op in self.ops[e]:
                need = {}
                need_sp = {}
                for d in op.deps:
                    if d is op:
                        continue
                    if d.eng == "sp":
                        j = d.idx % K
                        if d.idx > need_sp.get(j, -1):
                            need_sp[j] = d.idx
                        continue
                    if d.eng == e and e == "pe":
                        continue
                    if d.idx > need.get(d.eng, -1):
                        need[d.eng] = d.idx
                if e == "sp" and op.idx >= K:
                    j = op.idx % K
                    if op.idx - K > need_sp.get(j, -1):
                        need_sp[j] = op.idx - K
                lst = []
                for y, i in need.items():
                    if i > waited[y]:
                        lst.append((y, i))
                        waited[y] = i
                        self.ops[y][i].milestone = True
                for j, i in need_sp.items():
                    if i > waited_sp[j]:
                        lst.append(("sp", i))
                        waited_sp[j] = i
                plan[op] = lst
        for e in self.ENGS:
            if e == "sp":
                for op in self.ops[e]:
                    op.milestone = True
                    op.semval = 16 * (op.idx // K + 1)
                continue
            c = 0
            for op in self.ops[e]:
                if op.milestone:
                    c += 1
                    op.semval = c

        def semof(y, i):
            return sems["sp"][i % K] if y == "sp" else sems[y]

        def run_engine(e, eng):
            for op in self.ops[e]:
                for (y, i) in plan[op]:
                    eng.wait_ge(semof(y, i), self.ops[y][i].semval)
                ins = op.fn(eng)
                if op.milestone:
                    if e == "sp":
                        ins.then_inc(sems["sp"][op.idx % K], 16)
                    else:
                        ins.then_inc(sems[e], 1)
            if e == "sp":
                n = len(self.ops["sp"])
                for i in range(max(0, n - K), n):
                    eng.wait_ge(sems["sp"][i % K], self.ops["sp"][i].semval)

        @block.tensor
        def _(eng):
            run_engine("pe", eng)

        @block.scalar
        def _(eng):
            run_engine("act", eng)

        @block.vector
        def _(eng):
            run_engine("dve", eng)

        @block.gpsimd
        def _(eng):
            run_engine("pool", eng)

        @block.sync
        def _(eng):
            run_engine("sp", eng)


ARENA_KB = 207


def build(dbg=False, upto=None):
    nc = bass.Bass("TRN2", target_bir_lowering=False)

    def din(name, shape):
        return nc.dram_tensor(name, list(shape), F32, kind="ExternalInput").ap()

    x_d = din("x", [S, D])
    p_d = din("p", [S, 256])
    win_d = din("w_in", [D, 4096])
    wpool_d = din("w_pool", [4, 128, 128])
    pscale_d = din("pscale", [128, 4])
    wpu_d = din("w_pu", [512, D])
    wau_d = din("w_au", [512, D])
    wo_d = din("w_o", [D, D])
    ln1g_d = din("ln1_g", [D])
    ln1b_d = din("ln1_b", [D])
    wr_d = din("wr", [D, 36])
    br_d = din("br", [36])
    weg_d = din("w_eg", [NEXP, D, 512])
    weu_d = din("w_eu", [NEXP, D, 512])
    wed_d = din("w_ed", [NEXP, 512, D])
    wpg_d = din("w_pg", [D, D])
    wpp_d = din("w_pp", [256, D])
    ln2g_d = din("ln2_g", [D])
    ln2b_d = din("ln2_b", [D])
    cst_d = din("cst", [128, 448])
    out_d = nc.dram_tensor("out", [S, D], F32, kind="ExternalOutput").ap()
    dbg_outs = {}

    P = Prog()
    with ExitStack() as es:
        arena = es.enter_context(nc.sbuf_tensor("arena", [128, ARENA_KB * 256], F32))
        ps = es.enter_context(nc.psum_tensor("ps", [128, 4096], F32))
        sems = {e: es.enter_context(nc.semaphore("s_" + e)) for e in Prog.ENGS if e != "sp"}
        sems["sp"] = [es.enter_context(nc.semaphore("s_dma%d" % j)) for j in range(Prog.KDMA)]
        block = es.enter_context(nc.Block())

        def R(kb, nkb, dt=F32):
            a = arena[:, int(kb * 256):int((kb + nkb) * 256)]
            return a if dt == F32 else a.bitcast(dt)

        def bank(b, n=1):
            return ps[:, b * 512:(b + n) * 512]

        def cast(eng, out, in_, reads, writes):
            if eng == "act":
                P.add("act", lambda e: e.copy(out=out, in_=in_), reads=reads, writes=writes)
            else:
                P.add(eng, lambda e: e.tensor_copy(out=out, in_=in_), reads=reads, writes=writes)

        def dbg_dump(name, ap, reads):
            if not dbg:
                return
            shp = list(ap.shape)
            d = nc.dram_tensor("dbg_" + name, shp, ap.dtype, kind="ExternalOutput").ap()
            dbg_outs[name] = d
            n = shp[1]
            step = 2048
            for a in range(0, n, step):
                b = min(n, a + step)
                P.add("sp", lambda e, a=a, b=b: e.dma_start(out=d[:, a:b], in_=ap[:, a:b]), reads=reads)

        def phase(name):
            order = ["A", "B", "B1", "B2", "B3", "D", "E", "F", "F1", "F2", "F3", "F3b", "F3c", "F3d", "F4", "F5", "G", "I", "H", "J"]
            if upto is not None and order.index(name) > order.index(upto):
                P.enabled = False

        ident = R(0, 0.5)
        cstf = R(0.5, 1.75)
        cbf = R(2.25, 0.75, BF16).rearrange("p (a n) -> p a n", a=3)
        triU, ones_bf, mask01 = cbf[:, 0, :], cbf[:, 1, :], cbf[:, 2, :]
        rcs = cstf[:, 384:448].rearrange("p (g n) -> p g n", g=4)
        g_bc = R(3, 4)
        b_bc = R(7, 4)
        pscale = R(11, 0.0625)[:, 0:4]
        br_bc = R(11.0625, 0.1875)[:, 0:36]
        wpool_bf = R(11.25, 1, BF16).rearrange("p (g n) -> p g n", g=4)
        XT = R(14, 32, BF16).rearrange("p (k n) -> p k n", k=8)

        zeros_bf = R(12.25, 0.25, BF16)
        P.add("pool", lambda e: e.memset(zeros_bf, 0.0), writes=["zeros_bf"])
        identd = din("ident", [128, 128])
        P.add("sp", lambda e: e.dma_start(out=ident, in_=identd), writes=["ident"])
        P.add("sp", lambda e: e.dma_start(out=cstf, in_=cst_d), writes=["cstf"])
        P.add("pool", lambda e: e.tensor_copy(out=R(2.25, 0.75, BF16), in_=cstf[:, 0:384]), reads=["cstf"], writes=["cbf"])
        P.add("sp", lambda e: e.dma_start(out=pscale, in_=pscale_d), writes=["pscale"])
        P.add("sp", lambda e: e.dma_start(out=g_bc, in_=ln1g_d.partition_broadcast(128)), writes=["g_bc"])
        P.add("sp", lambda e: e.dma_start(out=b_bc, in_=ln1b_d.partition_broadcast(128)), writes=["b_bc"])
        P.add("sp", lambda e: e.dma_start(out=br_bc, in_=br_d.partition_broadcast(128)), writes=["br_bc"])

        PO = R(46, 16, BF16).rearrange("p (k n) -> p k n", k=4)
        AO = R(62, 16, BF16).rearrange("p (k n) -> p k n", k=4)
        QT = R(78, 16, BF16).rearrange("p (k n) -> p k n", k=4)
        NKTp = R(94, 32, BF16).rearrange("p (h n) -> p h n", h=8)
        Vp = R(126, 32, BF16).rearrange("p (i m r d) -> p i m r d", i=16, m=4, r=2)
        wst = [R(158, 16).rearrange("p (k n) -> p k n", k=8), R(174, 16).rearrange("p (k n) -> p k n", k=8)]
        wbf = [R(190, 8, BF16).rearrange("p (k n) -> p k n", k=8), R(198, 8, BF16).rearrange("p (k n) -> p k n", k=8)]

        def xT_keys(tc):
            return [("xT", 4 * tc + j) for j in range(4)]

        def load_win_block(cb, s, eng="pool"):
            P.add("sp", lambda e: e.dma_start(out=wst[s], in_=win_d[:, cb * 512:(cb + 1) * 512].rearrange("(k p) n -> p k n", p=128)),
                  writes=[("wst", s)])
            cast(eng, wbf[s], wst[s], [("wst", s)], [("wbf", s)])

        wpool_st2 = R(70, 2).rearrange("p (g n) -> p g n", g=4)
        P.add("sp", lambda e: e.dma_start(out=wpool_st2, in_=wpool_d.rearrange("g p n -> p g n")), writes=["wpool_st"])
        P.add("pool", lambda e: e.tensor_copy(out=wpool_bf, in_=wpool_st2), reads=["wpool_st"], writes=["wpool_bf"])

        load_win_block(0, 0, "act")
        load_win_block(1, 1, "act")
        xs = [R(62, 4), R(66, 4)]
        for i in range(NT):
            b = i % 2
            P.add("sp", lambda e, i=i, b=b: e.dma_start(out=xs[b], in_=x_d[i * 128:(i + 1) * 128, :]), writes=[("xs", b)])
            for k in range(8):
                P.add("pe", lambda e, k=k, b=b: e.transpose(out=ps[:, b * 1024 + k * 128: b * 1024 + (k + 1) * 128],
                                                            in_=xs[b][:, k * 128:(k + 1) * 128], identity=ident),
                      reads=[("xs", b), "ident"], writes=[("ps", 2 * b + k // 4)])
            P.add("act", lambda e, i=i, b=b: e.copy(out=XT[:, 0:4, i * 128:(i + 1) * 128],
                                                     in_=bank(2 * b).rearrange("p (k n) -> p k n", k=4)),
                  reads=[], writes=[("xT", i), ("ps", 2 * b)])
            P.add("dve", lambda e, i=i, b=b: e.tensor_copy(out=XT[:, 4:8, i * 128:(i + 1) * 128],
                                                            in_=bank(2 * b + 1).rearrange("p (k n) -> p k n", k=4)),
                  reads=[], writes=[("xT", i), ("ps", 2 * b + 1)])

        NKTp4 = R(94, 32, BF16).rearrange("p (m r n) -> p m r n", m=4, r=2)
        P.add("act", lambda e: e.memzero(NKTp4[64:128, :, 0, :]), writes=["NKTp"])
        P.add("act", lambda e: e.memzero(NKTp4[0:64, :, 1, :]), writes=["NKTp"])

        phase("B")
        phase("B1")
        up = R(126, 8)
        tmp = [R(134, 8), R(142, 8)]
        pooled = R(150, 4, BF16)
        pb_rot = [0]

        def next_bank():
            b = pb_rot[0] % 8
            pb_rot[0] += 1
            return b

        evac_rot = [0]

        def evac(out, in_, reads, writes, scale=None):
            evac_rot[0] += 1
            if evac_rot[0] % 2 == 0:
                if scale is None:
                    P.add("act", lambda e: e.copy(out=out, in_=in_), reads=reads, writes=writes)
                else:
                    P.add("act", lambda e: e.mul(out=out, in_=in_, mul=scale), reads=reads, writes=writes)
            else:
                if scale is None:
                    P.add("dve", lambda e: e.tensor_copy(out=out, in_=in_), reads=reads, writes=writes)
                else:
                    P.add("dve", lambda e: e.tensor_scalar(out=out, in0=in_, scalar1=scale, scalar2=None, op0=ALU.mult),
                          reads=reads, writes=writes)

        def proj_fm(wslot, mcols, tc, out_ap, out_key, scale=None):
            b = next_bank()
            for k in range(8):
                P.add("pe", lambda e, k=k, b=b: e.matmul(bank(b), lhsT=wbf[wslot][:, k, mcols[0]:mcols[1]],
                                                         rhs=XT[:, k, tc * 512:(tc + 1) * 512], start=(k == 0), stop=(k == 7)),
                      reads=[("wbf", wslot)] + xT_keys(tc), writes=[("ps", b)])
            evac(out_ap, bank(b), [("ps", b)], [out_key], scale)

        WIN = (2, 4, 8, 16)
        for g in range(4):
            for tc in range(4):
                proj_fm(0, (g * 128, (g + 1) * 128), tc, up[:, tc * 512:(tc + 1) * 512], "up")
            src, srck = up, "up"
            step = 1
            ti = 0
            while step < WIN[g]:
                dst, dstk = tmp[ti % 2], ("tmp", ti % 2)
                P.add("dve", lambda e, src=src, dst=dst, step=step: e.tensor_tensor(out=dst[:, step:], in0=src[:, step:], in1=src[:, :S - step], op=ALU.add),
                      reads=[srck], writes=[dstk])
                P.add("pool", lambda e, src=src, dst=dst, step=step: e.tensor_copy(out=dst[:, 0:step], in_=src[:, 0:step]),
                      reads=[srck], writes=[dstk])
                src, srck = dst, dstk
                step *= 2
                ti += 1
            w = WIN[g]
            P.add("dve", lambda e, src=src, w=w: e.scalar_tensor_tensor(out=pooled[:, 16:], in0=src[:, 16:], scalar=1.0 / w, in1=up[:, 16:],
                                                                         op0=ALU.mult, op1=ALU.subtract),
                  reads=[srck, "up"], writes=["pooled"])
            hd = tmp[ti % 2][:, 0:16]
            hdk = ("tmp", ti % 2)
            P.add("dve", lambda e, src=src, g=g, hd=hd: e.tensor_tensor(out=hd, in0=src[:, 0:16], in1=rcs[:, g, :], op=ALU.mult),
                  reads=[srck, "cstf"], writes=[hdk])
            P.add("dve", lambda e, hd=hd: e.tensor_tensor(out=pooled[:, 0:16], in0=hd, in1=up[:, 0:16], op=ALU.subtract),
                  reads=[hdk, "up"], writes=["pooled"])
            for tc in range(4):
                proj_fm(1, (g * 128, (g + 1) * 128), tc, QT[:, g, tc * 512:(tc + 1) * 512], "QT")
            for tc in range(4):
                b = next_bank()
                P.add("pe", lambda e, g=g, tc=tc, b=b: e.matmul(bank(b), lhsT=wpool_bf[:, g, :], rhs=pooled[:, tc * 512:(tc + 1) * 512], start=True, stop=True),
                      reads=["wpool_bf", "pooled"], writes=[("ps", b)])
                P.add("act", lambda e, g=g, tc=tc, b=b: e.activation(out=PO[:, g, tc * 512:(tc + 1) * 512], in_=bank(b), func=AF.Identity, scale=pscale[:, g:g + 1]),
                      reads=[("ps", b), "pscale"], writes=["PO"])
        dbg_dump("po", R(46, 16, BF16), ["PO"])

        phase("B2")
        load_win_block(2, 0, "act")
        for m in range(4):
            for tc in range(4):
                b = next_bank()
                for k in range(8):
                    P.add("pe", lambda e, k=k, b=b, m=m, tc=tc: e.matmul(bank(b), lhsT=wbf[0][:, k, m * 128:(m + 1) * 128],
                                                                       rhs=XT[:, k, tc * 512:(tc + 1) * 512], start=(k == 0), stop=(k == 7)),
                          reads=[("wbf", 0)] + xT_keys(tc), writes=[("ps", b)])
                P.add("act", lambda e, b=b, m=m, tc=tc: e.mul(out=NKTp[0:64, 2 * m, tc * 512:(tc + 1) * 512], in_=bank(b)[0:64, :], mul=-0.125),
                      reads=[], writes=["NKTp", ("ps", b)])
                P.add("dve", lambda e, b=b, m=m, tc=tc: e.tensor_scalar(out=NKTp[64:128, 2 * m + 1, tc * 512:(tc + 1) * 512], in0=bank(b)[64:128, :],
                                                                     scalar1=-0.125, scalar2=None, op0=ALU.mult),
                      reads=[], writes=["NKTp", ("ps", b)])
        phase("B3")
        P.add("act", lambda e: e.memzero(Vp[:, :, :, 0, 64:128]), writes=["Vp", "up", ("tmp", 0), ("tmp", 1), "pooled"])
        P.add("dve", lambda e: e.memset(Vp[:, :, :, 1, 0:64], 0.0), writes=["Vp", "up", ("tmp", 0), ("tmp", 1), "pooled"])
        load_win_block(3, 1, "act")
        for i in range(NT):
            b = next_bank()
            for k in range(8):
                P.add("pe", lambda e, k=k, b=b, i=i: e.matmul(bank(b), lhsT=XT[:, k, i * 128:(i + 1) * 128], rhs=wbf[1][:, k, :], start=(k == 0), stop=(k == 7)),
                      reads=[("wbf", 1), ("xT", i)], writes=[("ps", b)])
            src5 = bank(b).rearrange("p (m r d) -> p m r d", m=4, r=2)
            P.add("act", lambda e, i=i, src5=src5: e.copy(out=Vp[:, i, :, 0, 0:64], in_=src5[:, :, 0, :]), reads=[], writes=["Vp", ("ps", b)])
            P.add("dve", lambda e, i=i, src5=src5: e.tensor_copy(out=Vp[:, i, :, 1, 64:128], in_=src5[:, :, 1, :]), reads=[], writes=["Vp", ("ps", b)])
        P.barrier(lambda e: e.memset(R(206, 0.25), 0.0))

        phase("D")
        wpu_bf = R(176, 8, BF16).rearrange("p (k n) -> p k n", k=4)
        wau_bf = R(184, 8, BF16).rearrange("p (k n) -> p k n", k=4)
        wpst = R(192, 8).rearrange("p (k n) -> p k n", k=2)
        prefetch_jobs = [(wd_, wb_, wk_, hk) for (wd_, wb_, wk_) in ((wpu_d, wpu_bf, "wpu_bf"), (wau_d, wau_bf, "wau_bf")) for hk in range(2)]

        def prefetch_job(j):
            if j >= 1:
                wd_, wb_, wk_, hk = prefetch_jobs[j - 1]
                P.add("dve", lambda e: e.tensor_copy(out=wb_[:, 2 * hk:2 * hk + 2, :], in_=wpst), reads=["wpst"], writes=[wk_])
            if j < len(prefetch_jobs):
                wd2, wb2, wk2, hk2 = prefetch_jobs[j]
                P.add("sp", lambda e: e.dma_start(out=wpst, in_=wd2[hk2 * 256:(hk2 + 1) * 256, :].rearrange("(k p) n -> p k n", p=128)), writes=["wpst"])
        Eb = [R(158, 4), R(162, 4)]
        SPb = [R(166, 2, BF16), R(168, 2, BF16)]
        Sb = R(170, 2, BF16)
        Ab = [R(172, 2, BF16), R(174, 2, BF16)]
        Zs = ps[:, 0:1024]
        PBs = [ps[:, 1024:2048], ps[:, 2048:3072]]
        Op_ = ps[:, 3072:4096]

        units = []
        for h in range(8):
            for hf in range(2):
                for kb in range(8 * hf + 7, -1, -1):
                    units.append((h, hf, kb))

        def pieces(c0):
            out = []
            if c0 < 512:
                out.append((c0, 512))
                out.append((512, 1024))
            else:
                out.append((c0, 1024))
            return out

        def stage1(u, ui):
            h, hf, kb = u
            m, pb = h // 2, (h % 2) * 64
            zb = ui % 2
            c0 = max(kb * 128 - hf * 1024, 0)
            diag = kb * 128 >= hf * 1024
            for (a, b) in pieces(c0):
                P.add("pe", lambda e, a=a, b=b: e.matmul(Zs[:, a:b], lhsT=NKTp[:, h, kb * 128:(kb + 1) * 128],
                                                         rhs=QT[:, m, hf * 1024 + a: hf * 1024 + b], start=True, stop=True),
                      reads=["NKTp", "QT"], writes=["Z"])
            P.add("act", lambda e: e.activation(out=Eb[zb][:, c0:], in_=Zs[:, c0:], func=AF.Exp, scale=-1.0),
                  reads=["Z"], writes=[("E", zb)])

        def stage1b(u, ui):
            h, hf, kb = u
            zb = ui % 2
            c0 = max(kb * 128 - hf * 1024, 0)
            diag = kb * 128 >= hf * 1024
            P.add("act", lambda e: e.activation(out=SPb[zb][:, c0:], in_=Eb[zb][:, c0:], func=AF.Ln, bias=1.0),
                  reads=[("E", zb)], writes=[("SP", zb)])
            if diag:
                P.add("dve", lambda e: e.tensor_tensor(out=SPb[zb][:, c0:c0 + 128], in0=SPb[zb][:, c0:c0 + 128], in1=mask01, op=ALU.mult),
                      reads=[("SP", zb), "cbf"], writes=[("SP", zb)])

        def stage2(u, ui):
            h, hf, kb = u
            m, pb = h // 2, (h % 2) * 64
            zb = ui % 2
            c0 = max(kb * 128 - hf * 1024, 0)
            diag = kb * 128 >= hf * 1024
            first = (kb == 8 * hf + 7)
            if first:
                P.add("dve", lambda e: e.memset(Sb, 0.0), writes=["S"])
            for (a, b) in pieces(c0):
                P.add("pe", lambda e, a=a, b=b: e.matmul(PBs[zb][:, a:b], lhsT=triU, rhs=SPb[zb][:, a:b], start=True, stop=False),
                      reads=[("SP", zb), "cbf"], writes=[("PB", zb)])
                if not first:
                    P.add("pe", lambda e, a=a, b=b: e.matmul(PBs[zb][:, a:b], lhsT=ones_bf, rhs=Sb[:, a:b], start=False, stop=False),
                          reads=["S", "cbf"], writes=[("PB", zb)])
                P.add("pe", lambda e, a=a, b=b: e.matmul(PBs[zb][:, a:b], lhsT=NKTp[:, h, kb * 128:(kb + 1) * 128],
                                                         rhs=QT[:, m, hf * 1024 + a: hf * 1024 + b], start=False, stop=True),
                      reads=["NKTp", "QT"], writes=[("PB", zb)])
            P.add("act", lambda e: e.activation(out=Ab[zb][:, c0:], in_=PBs[zb][:, c0:], func=AF.Exp, scale=-1.0),
                  reads=[("PB", zb)], writes=[("A", zb)])
            if diag:
                P.add("dve", lambda e: e.tensor_tensor(out=Ab[zb][:, c0:c0 + 128], in0=Ab[zb][:, c0:c0 + 128], in1=mask01, op=ALU.mult),
                      reads=[("A", zb), "cbf"], writes=[("A", zb)])
            if kb > 0:
                P.add("dve", lambda e: e.tensor_tensor(out=Sb[:, c0:], in0=Sb[:, c0:], in1=SPb[zb][:, c0:], op=ALU.add),
                      reads=["S", ("SP", zb)], writes=["S"])

        def stage3(u, ui):
            h, hf, kb = u
            m, pb = h // 2, (h % 2) * 64
            zb = ui % 2
            c0 = max(kb * 128 - hf * 1024, 0)
            if kb == 8 * hf + 7:
                for j in range(2):
                    P.add("pe", lambda e, j=j: e.matmul(Op_[:, j * 512:(j + 1) * 512], lhsT=zeros_bf, rhs=QT[:, 0, 0:512],
                                                        start=True, stop=False, skip_group_check=True),
                          reads=["zeros_bf", "QT"], writes=["O"])
            for (a, b) in pieces(c0):
                P.add("pe", lambda e, a=a, b=b: e.matmul(Op_[:, a:b], lhsT=Vp[:, kb, m, h % 2, :], rhs=Ab[zb][:, a:b],
                                                         start=False, stop=(kb == 0), skip_group_check=True),
                      reads=["Vp", ("A", zb)], writes=["O"])
            if kb == 0:
                P.add("dve", lambda e: e.tensor_copy(out=AO[pb:pb + 64, m, hf * 1024:(hf + 1) * 1024], in_=Op_[pb:pb + 64, :]),
                      reads=["O"], writes=["AO"])

        nu = len(units)
        for it in range(nu + 2):
            if it < nu:
                stage1(units[it], it)
                stage1b(units[it], it)
            if 0 <= it - 1 < nu:
                stage2(units[it - 1], it - 1)
            if 0 <= it - 2 < nu:
                stage3(units[it - 2], it - 2)
            if it % 12 == 11 and it // 12 <= len(prefetch_jobs):
                prefetch_job(it // 12)
        dbg_dump("ao", R(62, 16, BF16), ["AO"])
        P.barrier(lambda e: e.memset(R(206, 0.25), 0.0))

        phase("E")
        MG = R(78, 32, BF16).rearrange("p (k n) -> p k n", k=8)
        gw = [R(110 + 8 * j, 8, BF16).rearrange("p (k n) -> p k n", k=8) for j in range(4)]
        gst = [R(142, 16), R(158, 16)]
        etmp = [[R(192 + 4 * j + 2 * q, 2) for q in range(2)] for j in range(2)]


        def load_gate_block(cb, slot, s):
            P.add("sp", lambda e: e.dma_start(out=gst[s].rearrange("p (k n) -> p k n", k=8),
                                              in_=win_d[:, cb * 512:(cb + 1) * 512].rearrange("(k p) n -> p k n", p=128)), writes=[("gst", s)])
            cast(("act", "dve", "act", "dve")[slot], gw[slot], gst[s].rearrange("p (k n) -> p k n", k=8), [("gst", s)], [("gw", slot)])

        load_gate_block(4, 0, 0)
        load_gate_block(6, 1, 1)
        load_gate_block(5, 2, 0)
        load_gate_block(7, 3, 1)
        for hh in range(2):
            P.add("sp", lambda e, hh=hh: e.dma_start(out=gst[hh].rearrange("p (k n) -> p k n", k=8),
                                                     in_=wo_d[:, hh * 512:(hh + 1) * 512].rearrange("(k p) n -> p k n", p=128)), writes=[("gst", hh)])
        it = 0
        for pair in range(2):
            for cc in range(4):
                c = pair * 4 + cc
                for tc in range(4):
                    pbs = (it % 2) * 4
                    tb = etmp[it % 2]
                    it += 1
                    for (bi, slot) in ((0, 2 * pair), (1, 2 * pair + 1)):
                        for k in range(8):
                            P.add("pe", lambda e, k=k, bi=bi, slot=slot, pbs=pbs, cc=cc, tc=tc: e.matmul(
                                bank(pbs + bi), lhsT=gw[slot][:, k, cc * 128:(cc + 1) * 128], rhs=XT[:, k, tc * 512:(tc + 1) * 512],
                                start=(k == 0), stop=(k == 7)),
                                reads=[("gw", slot)] + xT_keys(tc), writes=[("ps", pbs + bi)])
                    for (bi, wsb, src, key, wkey) in ((2, wpu_bf, PO, "PO", "wpu_bf"), (3, wau_bf, AO, "AO", "wau_bf")):
                        for k in range(4):
                            P.add("pe", lambda e, k=k, bi=bi, wsb=wsb, src=src, pbs=pbs, c=c, tc=tc: e.matmul(
                                bank(pbs + bi), lhsT=wsb[:, k, c * 128:(c + 1) * 128], rhs=src[:, k, tc * 512:(tc + 1) * 512],
                                start=(k == 0), stop=(k == 3)),
                                reads=[key, wkey], writes=[("ps", pbs + bi)])
                    P.add("act", lambda e, pbs=pbs, tb=tb: e.activation(out=tb[0], in_=bank(pbs + 0), func=AF.Sigmoid),
                          reads=[("ps", pbs + 0)], writes=[("et", it % 2, 0)])
                    P.add("act", lambda e, pbs=pbs, tb=tb: e.activation(out=tb[1], in_=bank(pbs + 1), func=AF.Sigmoid),
                          reads=[("ps", pbs + 1)], writes=[("et", it % 2, 1)])
                    P.add("dve", lambda e, pbs=pbs, tb=tb: e.tensor_tensor(out=tb[0], in0=bank(pbs + 2), in1=tb[0], op=ALU.mult),
                          reads=[("ps", pbs + 2), ("et", it % 2, 0)], writes=[("et", it % 2, 0)])
                    P.add("dve", lambda e, pbs=pbs, tb=tb: e.tensor_tensor(out=tb[1], in0=bank(pbs + 3), in1=tb[1], op=ALU.mult),
                          reads=[("ps", pbs + 3), ("et", it % 2, 1)], writes=[("et", it % 2, 1)])
                    P.add("pool", lambda e, tb=tb, c=c, tc=tc: e.tensor_tensor(out=MG[:, c, tc * 512:(tc + 1) * 512], in0=tb[0], in1=tb[1], op=ALU.add),
                          reads=[("et", it % 2, 0), ("et", it % 2, 1)], writes=["MG"])
        dbg_dump("mg", R(78, 32, BF16), ["MG"])
        P.barrier(lambda e: e.memset(R(206, 0.25), 0.0))

        phase("F")
        ACC = R(110, 64).rearrange("p (t n) -> p t n", t=NT)
        wo_bf = R(174, 16, BF16).rearrange("p (k n) -> p k n", k=8)
        wo_st = R(190, 16).rearrange("p (k n) -> p k n", k=8)
        xr = [R(46, 4), R(50, 4)]
        rbufs = [R(54, 4), R(190, 4)]
        x1bs = [R(58, 4), R(194, 4)]
        sqb = R(62, 2, BF16)
        x1Tfs = [R(64, 4).rearrange("p (k n) -> p k n", k=8), R(198, 4).rearrange("p (k n) -> p k n", k=8)]
        LOG = R(68, 2.25).rearrange("p (t n) -> p t n", t=NT)
        wr_sb = R(70.5, 1.125).rearrange("p (k n) -> p k n", k=8)
        CMB = R(76, 2).rearrange("p (t n) -> p t n", t=NT)
        STT = R(205.5, 0.5).rearrange("p (t n) -> p t n", t=NT)
        RTS = R(62, 3.5)

        for hh in range(2):
            cast(("act", "dve")[hh], wo_bf[:, :, hh * 512:(hh + 1) * 512], gst[hh].rearrange("p (k n) -> p k n", k=8), [("gst", hh)], [("wo_bf", hh)])
        P.add("sp", lambda e: e.dma_start(out=wr_sb, in_=wr_d.rearrange("(k p) n -> p k n", p=128)), writes=["wr_sb"])

        def ln_stats(i, src, srck, stats, sq, sqk):
            P.add("act", lambda e: e.activation(out=sq, in_=src, func=AF.Square, accum_out=stats[:, 1:2]),
                  reads=[srck], writes=[sqk, ("st", i, 1)])
            P.add("dve", lambda e: e.tensor_scalar(out=stats[:, 2:3], in0=stats[:, 0:1], scalar1=1.0 / D, scalar2=None, op0=ALU.mult),
                  reads=[("st", i, 0)], writes=[("st", i, 2)])
            P.add("dve", lambda e: e.tensor_tensor(out=stats[:, 3:4], in0=stats[:, 2:3], in1=stats[:, 2:3], op=ALU.mult),
                  reads=[("st", i, 2)], writes=[("st", i, 3)])
            P.add("dve", lambda e: e.scalar_tensor_tensor(out=stats[:, 4:5], in0=stats[:, 1:2], scalar=1.0 / D, in1=stats[:, 3:4], op0=ALU.mult, op1=ALU.subtract),
                  reads=[("st", i, 1), ("st", i, 3)], writes=[("st", i, 4)])
            P.add("act", lambda e: e.activation(out=stats[:, 5:6], in_=stats[:, 4:5], func=AF.Ln, bias=EPS),
                  reads=[("st", i, 4)], writes=[("st", i, 5)])
            P.add("act", lambda e: e.activation(out=stats[:, 6:7], in_=stats[:, 5:6], func=AF.Exp, scale=-0.5),
                  reads=[("st", i, 5)], writes=[("st", i, 6)])
            P.add("dve", lambda e: e.scalar_tensor_tensor(out=stats[:, 7:8], in0=stats[:, 2:3], scalar=-1.0, in1=stats[:, 6:7], op0=ALU.mult, op1=ALU.mult),
                  reads=[("st", i, 2), ("st", i, 6)], writes=[("st", i, 7)])

        def f_vars(i):
            b = i % 2
            return (b, ps[:, b * 1024:(b + 1) * 1024], rbufs[b], x1bs[b], x1Tfs[b], ("rbuf", b), ("x1b", b), ("x1Tf", b),
                    [], STT[:, i, :])

        def f_stage_a(i):
            b, yb, rbuf, x1b, x1Tf, rk, xk, tk, alias, st = f_vars(i)
            P.add("sp", lambda e: e.dma_start(out=xr[b], in_=x_d[i * 128:(i + 1) * 128, :]), writes=[("xr", b)])
            for hh in range(2):
                for k in range(8):
                    P.add("pe", lambda e, hh=hh, k=k: e.matmul(yb[:, hh * 512:(hh + 1) * 512], lhsT=MG[:, k, i * 128:(i + 1) * 128],
                                                             rhs=wo_bf[:, k, hh * 512:(hh + 1) * 512], start=(k == 0), stop=(k == 7)),
                          reads=["MG", ("wo_bf", hh)], writes=[("Y", b)])
            P.add("dve", lambda e: e.scalar_tensor_tensor(out=rbuf, in0=xr[b], scalar=ALPHA, in1=yb, op0=ALU.mult, op1=ALU.add, accum_out=st[:, 0:1]),
                  reads=[("xr", b)], writes=[rk, ("st", i, 0), ("Y", b)] + alias)

        def f_stage_b(i):
            b, yb, rbuf, x1b, x1Tf, rk, xk, tk, alias, st = f_vars(i)
            ln_stats(i, rbuf, rk, st, sqb, "sqb")
            P.add("act", lambda e: e.activation(out=x1b, in_=rbuf, func=AF.Identity, scale=st[:, 6:7], bias=st[:, 7:8]),
                  reads=[rk, ("st", i, 6), ("st", i, 7)], writes=[xk] + alias)
            P.add("dve", lambda e: e.tensor_tensor(out=x1b, in0=x1b, in1=g_bc, op=ALU.mult), reads=[xk, "g_bc"], writes=[xk])
            P.add("dve", lambda e: e.tensor_tensor(out=x1b, in0=x1b, in1=b_bc, op=ALU.add), reads=[xk, "b_bc"], writes=[xk])

        def f_stage_c(i):
            b, yb, rbuf, x1b, x1Tf, rk, xk, tk, alias, st = f_vars(i)
            P.add("act", lambda e: e.mul(out=ACC[:, i, :], in_=x1b, mul=ALPHA), reads=[xk], writes=[("ACC", i)])
            for k in range(8):
                P.add("pe", lambda e, k=k: e.transpose(out=ps[:, 2048 + k * 128: 2048 + (k + 1) * 128], in_=x1b[:, k * 128:(k + 1) * 128], identity=ident),
                      reads=[xk, "ident"], writes=[("psT", k // 4)])
            for hh in range(2):
                src3 = bank(4 + hh).rearrange("p (k n) -> p k n", k=4)
                P.add("act", lambda e, hh=hh, src3=src3: e.copy(out=XT[:, 4 * hh:4 * hh + 4, i * 128:(i + 1) * 128], in_=src3),
                      reads=[], writes=[("xT", i), ("psT", hh)])
                P.add("dve", lambda e, hh=hh, src3=src3: e.tensor_copy(out=x1Tf[:, 4 * hh:4 * hh + 4, :], in_=src3),
                      reads=[], writes=[tk, ("psT", hh)] + alias)
            for k in range(8):
                P.add("pe", lambda e, k=k: e.matmul(ps[:, 3072:3108], lhsT=x1Tf[:, k, :], rhs=wr_sb[:, k, :], start=(k == 0), stop=(k == 7)),
                      reads=[tk, "wr_sb"], writes=["psL"])

        def f_stage_d(i):
            P.add("dve", lambda e: e.tensor_tensor(out=LOG[:, i, :], in0=ps[:, 3072:3108], in1=br_bc, op=ALU.add),
                  reads=["br_bc"], writes=["LOG", "psL"])

        psts = [R(78, 16), R(94, 16)]
        for n in range(NT + 2):
            if n < NT:
                f_stage_a(n)
            if n == NT - 1:
                for hh in range(2):
                    P.add("sp", lambda e, hh=hh: e.dma_start(out=psts[hh].rearrange("p (k n) -> p k n", k=8),
                                                             in_=wpg_d[:, hh * 512:(hh + 1) * 512].rearrange("(k p) n -> p k n", p=128)),
                          writes=[("pst", hh), "MG"])
            if 0 <= n - 2 < NT:
                f_stage_c(n - 2)
            if 0 <= n - 1 < NT:
                f_stage_b(n - 1)
            if 0 <= n - 2 < NT:
                f_stage_d(n - 2)
        P.add("sp", lambda e: e.dma_start(out=g_bc, in_=ln2g_d.partition_broadcast(128)), writes=["g_bc"])
        P.add("sp", lambda e: e.dma_start(out=b_bc, in_=ln2b_d.partition_broadcast(128)), writes=["b_bc"])
        if dbg:
            dbg_dump("acc1", R(110, 64), [("ACC", i) for i in range(NT)])
            dbg_dump("log", R(68, 2.25), ["LOG"])

        phase("G")
        def rt(off, n, shape3=None):
            a = RTS[:, off:off + n]
            return a
        T = NT
        gl = LOG[:, :, 0:4]
        el = LOG[:, :, 4:36].rearrange("p t (g e) -> p t g e", g=4)
        gm = RTS[:, 0:16]
        gsum = RTS[:, 16:32]
        gp = RTS[:, 32:48]
        m1 = RTS[:, 48:64]
        m2 = RTS[:, 64:80]
        dd = RTS[:, 80:96]
        w1g = RTS[:, 96:112]
        w2g = RTS[:, 112:128]
        gsub = RTS[:, 128:192].rearrange("p (t n) -> p t n", t=T)
        gmask = RTS[:, 192:256].rearrange("p (t n) -> p t n", t=T)
        sel = RTS[:, 256:384].rearrange("p (t n) -> p t n", t=T)
        sel2 = RTS[:, 384:512].rearrange("p (t n) -> p t n", t=T)
        mk1 = RTS[:, 512:640].rearrange("p (t n) -> p t n", t=T)
        mk2 = RTS[:, 640:768].rearrange("p (t n) -> p t n", t=T)
        tmp8 = RTS[:, 768:896].rearrange("p (t n) -> p t n", t=T)

        def bc(a2, n):
            return a2.unsqueeze(2).to_broadcast([128, T, n])

        router_ops = []

        def dv(fn, reads, writes, eng="dve"):
            router_ops.append((eng, fn, reads, writes))
        dv(lambda e: e.reduce_max(out=gm, in_=gl, axis=AX.X), ["LOG"], ["gm"])
        dv(lambda e: e.tensor_tensor(out=gsub, in0=gl, in1=bc(gm, 4), op=ALU.subtract), ["LOG", "gm"], ["gsub"])
        router_ops.append(("act", lambda e: e.activation(out=gsub, in_=gsub, func=AF.Exp), ["gsub"], ["gsub"]))
        dv(lambda e: e.reduce_sum(out=gsum, in_=gsub, axis=AX.X), ["gsub"], ["gsum"])
        dv(lambda e: e.reciprocal(out=gp, in_=gsum), ["gsum"], ["gp"], eng="dve")
        dv(lambda e: e.tensor_tensor(out=gmask, in0=gl, in1=bc(gm, 4), op=ALU.is_equal), ["LOG", "gm"], ["gmask"])
        dv(lambda e: e.tensor_tensor(out=sel, in0=el[:, :, 0, :], in1=gmask[:, :, 0:1].to_broadcast([128, T, 8]), op=ALU.mult), ["LOG", "gmask"], ["sel"])
        for g in range(1, 4):
            dv(lambda e, g=g: e.tensor_tensor(out=tmp8, in0=el[:, :, g, :], in1=gmask[:, :, g:g + 1].to_broadcast([128, T, 8]), op=ALU.mult), ["LOG", "gmask"], ["tmp8"])
            dv(lambda e: e.tensor_tensor(out=sel, in0=sel, in1=tmp8, op=ALU.add), ["sel", "tmp8"], ["sel"])
        dv(lambda e: e.reduce_max(out=m1, in_=sel, axis=AX.X), ["sel"], ["m1"])
        dv(lambda e: e.tensor_tensor(out=mk1, in0=sel, in1=bc(m1, 8), op=ALU.is_equal), ["sel", "m1"], ["mk1"])
        dv(lambda e: e.scalar_tensor_tensor(out=sel2, in0=mk1, scalar=-1e30, in1=sel, op0=ALU.mult, op1=ALU.add), ["sel", "mk1"], ["sel2"])
        dv(lambda e: e.reduce_max(out=m2, in_=sel2, axis=AX.X), ["sel2"], ["m2"])
        dv(lambda e: e.tensor_tensor(out=mk2, in0=sel2, in1=bc(m2, 8), op=ALU.is_equal), ["sel2", "m2"], ["mk2"])
        dv(lambda e: e.tensor_tensor(out=dd, in0=m2, in1=m1, op=ALU.subtract), ["m1", "m2"], ["dd"])
        router_ops.append(("act", lambda e: e.activation(out=dd, in_=dd, func=AF.Exp), ["dd"], ["dd"]))
        dv(lambda e: e.tensor_scalar(out=dd, in0=dd, scalar1=1.0, scalar2=None, op0=ALU.add), ["dd"], ["dd"])
        dv(lambda e: e.reciprocal(out=w1g, in_=dd), ["dd"], ["w1g"], eng="dve")
        dv(lambda e: e.tensor_tensor(out=w1g, in0=w1g, in1=gp, op=ALU.mult), ["w1g", "gp"], ["w1g"])
        dv(lambda e: e.tensor_tensor(out=w2g, in0=gp, in1=w1g, op=ALU.subtract), ["w1g", "gp"], ["w2g"])
        dv(lambda e: e.tensor_tensor(out=sel, in0=mk1, in1=bc(w1g, 8), op=ALU.mult), ["mk1", "w1g", "sel2"], ["sel"])
        dv(lambda e: e.tensor_tensor(out=tmp8, in0=mk2, in1=bc(w2g, 8), op=ALU.mult), ["mk2", "w2g"], ["tmp8"])
        dv(lambda e: e.tensor_tensor(out=sel, in0=sel, in1=tmp8, op=ALU.add), ["sel", "tmp8"], ["sel"])
        for g in range(4):
            dv(lambda e, g=g: e.tensor_tensor(out=CMB[:, :, 8 * g:8 * g + 8], in0=sel, in1=gmask[:, :, g:g + 1].to_broadcast([128, T, 8]), op=ALU.mult),
               ["sel", "gmask"], ["CMB"])
        P.barrier(lambda e: e.memset(R(206, 0.25), 0.0))
        phase("I")
        wpg_bf = R(174, 16, BF16).rearrange("p (k n) -> p k n", k=8)
        wpp_bf = R(102, 4, BF16).rearrange("p (k n) -> p k n", k=2)
        wppst = R(106, 4).rearrange("p (k n) -> p k n", k=2)
        ptile = [R(54, 1), R(55, 1)]
        pT = [R(56, 0.5, BF16).rearrange("p (k n) -> p k n", k=2), R(56.5, 0.5, BF16).rearrange("p (k n) -> p k n", k=2)]
        sgb = [R(57, 2), R(59, 2)]
        for hh in range(2):
            cast(("act", "dve")[hh], wpg_bf[:, :, hh * 512:(hh + 1) * 512], psts[hh].rearrange("p (k n) -> p k n", k=8), [("pst", hh)], [("wpg_bf", hh)])
        for hc in range(2):
            P.add("sp", lambda e, hc=hc: e.dma_start(out=wppst, in_=wpp_d[:, hc * 512:(hc + 1) * 512].rearrange("(k p) n -> p k n", p=128)),
                  writes=["wppst", ("pst", 1)])
            P.add("pool", lambda e, hc=hc: e.tensor_copy(out=wpp_bf[:, :, hc * 512:(hc + 1) * 512], in_=wppst), reads=["wppst"], writes=["wpp_bf"])

        phase("H")
        slots = [R(78 + 8 * j, 8, BF16) for j in range(4)] + [R(174, 8, BF16), R(182, 8, BF16)]
        stg = [R(190, 8), R(198, 8), R(46, 8)]
        hT = [R(54, 4, BF16).rearrange("p (k n) -> p k n", k=4), R(58, 4, BF16).rearrange("p (k n) -> p k n", k=4)]
        sgx = [R(62, 2), R(64, 2)]
        stg_rot = [0]

        def wviews(e_):
            s0 = (e_ % 2) * 3
            wg = slots[s0].rearrange("p (k n) -> p k n", k=8)
            wu = slots[s0 + 1].rearrange("p (k n) -> p k n", k=8)
            wd = slots[s0 + 2].rearrange("p (k n) -> p k n", k=4)
            return wg, wu, wd, s0

        def load_expert(e_):
            wg, wu, wd, s0 = wviews(e_)
            jobs = []
            for (dsrc, dstv, sk) in ((weg_d, wg, s0), (weu_d, wu, s0 + 1)):
                for hk in range(2):
                    jobs.append((dsrc[e_, hk * 512:(hk + 1) * 512, :].rearrange("(k p) n -> p k n", p=128), dstv[:, 4 * hk:4 * hk + 4, :], sk))
            for hc in range(2):
                jobs.append((wed_d[e_, :, hc * 512:(hc + 1) * 512].rearrange("(k p) n -> p k n", p=128), wd[:, :, hc * 512:(hc + 1) * 512], s0 + 2))
            for (src, dst, sk) in jobs:
                s = stg_rot[0] % 3
                stg_rot[0] += 1
                sv = stg[s].rearrange("p (k n) -> p k n", k=4)
                P.add("sp", lambda e, src=src, sv=sv: e.dma_start(out=sv, in_=src), writes=[("stg", s)])
                alias = []
                if e_ == 0:
                    alias = [("pst", 0), ("pst", 1)]
                elif e_ == 1:
                    alias = ["wpp_bf", "wppst"] if sk == 3 else [("wpg_bf", 0), ("wpg_bf", 1)]
                cast("act" if e_ == 0 else "pool", dst, sv, [("stg", s)], [("slot", sk)] + alias)

        GB = [0, 1]
        UB = [2, 3]
        DB = [4, 5, 6, 7]
        gu_rot = [0]
        d_rot = [0]

        def gu_step(e_, tc):
            wg, wu, wd, s0 = wviews(e_)
            hb = tc % 2
            for dc in range(4):
                q = gu_rot[0] % 2
                gu_rot[0] += 1
                for (bk, wv, sk) in ((GB[q], wg, s0), (UB[q], wu, s0 + 1)):
                    for k in range(8):
                        P.add("pe", lambda e, k=k, bk=bk, wv=wv, dc=dc, tc=tc: e.matmul(bank(bk), lhsT=wv[:, k, dc * 128:(dc + 1) * 128], rhs=XT[:, k, tc * 512:(tc + 1) * 512],
                                                                                   start=(k == 0), stop=(k == 7)),
                              reads=[("slot", sk)] + xT_keys(tc), writes=[("ps", bk)])
                ax = ["CMB"] if (e_ == 0 and tc == 0 and dc < 2) else []
                ah = ([("ptile", 0), ("ptile", 1), ("pT", 0), ("pT", 1), ("sgb", 0), ("sgb", 1)] if (e_ == 0 and tc < 2 and dc == 0) else [])
                P.add("act", lambda e, q=q: e.activation(out=sgx[q], in_=bank(GB[q]), func=AF.Silu), reads=[("ps", GB[q])], writes=[("sgx", q)] + ax)
                P.add("dve", lambda e, q=q, hb=hb, dc=dc: e.tensor_tensor(out=hT[hb][:, dc, :], in0=bank(UB[q]), in1=sgx[q], op=ALU.mult),
                      reads=[("ps", UB[q]), ("sgx", q)], writes=[("hT", hb)] + ah)

        def down_step(e_, tc):
            wg, wu, wd, s0 = wviews(e_)
            hb = tc % 2
            for ti in range(4):
                i = tc * 4 + ti
                for hh in range(2):
                    bk = DB[d_rot[0] % 4]
                    d_rot[0] += 1
                    for dc in range(4):
                        P.add("pe", lambda e, dc=dc, bk=bk, ti=ti, hh=hh, hb=hb, wd=wd: e.matmul(bank(bk), lhsT=hT[hb][:, dc, ti * 128:(ti + 1) * 128],
                                                                                           rhs=wd[:, dc, hh * 512:(hh + 1) * 512], start=(dc == 0), stop=(dc == 3)),
                              reads=[("hT", hb), ("slot", s0 + 2)], writes=[("ps", bk)])
                    P.add("dve", lambda e, bk=bk, i=i, hh=hh, e_=e_: e.scalar_tensor_tensor(out=ACC[:, i, hh * 512:(hh + 1) * 512], in0=bank(bk), scalar=CMB[:, i, e_:e_ + 1],
                                                                                          in1=ACC[:, i, hh * 512:(hh + 1) * 512], op0=ALU.mult, op1=ALU.add),
                          reads=[("ps", bk), "CMB", ("ACC", i)], writes=[("ACC", i)])

        ob = [R(66, 4), R(70, 4)]
        sqjunk = R(0.5, 2, BF16)
        STT2 = R(74, 0.5).rearrange("p (t n) -> p t n", t=NT)
        def j_s1(i):
            b = i % 2
            st = STT2[:, i, :]
            acc_i = ACC[:, i, :]
            P.add("act", lambda e: e.activation(out=sqjunk, in_=acc_i, func=AF.Identity, accum_out=st[:, 0:1]),
                  reads=[("ACC", i)], writes=["sqjunk", ("st2", i, 0)])
            P.add("act", lambda e: e.activation(out=sqjunk, in_=acc_i, func=AF.Square, accum_out=st[:, 1:2]),
                  reads=[("ACC", i)], writes=["sqjunk", ("st2", i, 1)])
            P.add("pool", lambda e: e.tensor_scalar(out=st[:, 2:3], in0=st[:, 0:1], scalar1=1.0 / D, scalar2=None, op0=ALU.mult),
                  reads=[("st2", i, 0)], writes=[("st2", i, 2)])
            P.add("pool", lambda e: e.tensor_tensor(out=st[:, 3:4], in0=st[:, 2:3], in1=st[:, 2:3], op=ALU.mult),
                  reads=[("st2", i, 2)], writes=[("st2", i, 3)])
            P.add("pool", lambda e: e.tensor_scalar(out=st[:, 4:5], in0=st[:, 1:2], scalar1=1.0 / D, scalar2=None, op0=ALU.mult),
                  reads=[("st2", i, 1)], writes=[("st2", i, 4)])
            P.add("pool", lambda e: e.tensor_tensor(out=st[:, 4:5], in0=st[:, 4:5], in1=st[:, 3:4], op=ALU.subtract),
                  reads=[("st2", i, 4), ("st2", i, 3)], writes=[("st2", i, 4)])
            P.add("act", lambda e: e.activation(out=st[:, 5:6], in_=st[:, 4:5], func=AF.Ln, bias=EPS),
                  reads=[("st2", i, 4)], writes=[("st2", i, 5)])
            P.add("act", lambda e: e.activation(out=st[:, 6:7], in_=st[:, 5:6], func=AF.Exp, scale=-0.5),
                  reads=[("st2", i, 5)], writes=[("st2", i, 6)])
            P.add("pool", lambda e: e.tensor_tensor(out=st[:, 7:8], in0=st[:, 2:3], in1=st[:, 6:7], op=ALU.mult),
                  reads=[("st2", i, 2), ("st2", i, 6)], writes=[("st2", i, 7)])
            P.add("pool", lambda e: e.tensor_scalar(out=st[:, 7:8], in0=st[:, 7:8], scalar1=-1.0, scalar2=None, op0=ALU.mult),
                  reads=[("st2", i, 7)], writes=[("st2", i, 7)])

        def j_s2(i):
            b = i % 2
            st = STT2[:, i, :]
            acc_i = ACC[:, i, :]
            P.add("act", lambda e: e.activation(out=ob[b], in_=acc_i, func=AF.Identity, scale=st[:, 6:7], bias=st[:, 7:8]),
                  reads=[("ACC", i), ("st2", i, 6), ("st2", i, 7)], writes=[("ob", b)])
            P.add("dve", lambda e: e.tensor_tensor(out=ob[b], in0=ob[b], in1=g_bc, op=ALU.mult), reads=[("ob", b), "g_bc"], writes=[("ob", b)])
            P.add("dve", lambda e: e.tensor_tensor(out=ob[b], in0=ob[b], in1=b_bc, op=ALU.add), reads=[("ob", b), "b_bc"], writes=[("ob", b)])
            P.add("sp", lambda e: e.dma_start(out=out_d[i * 128:(i + 1) * 128, :], in_=ob[b]), reads=[("ob", b)])


        ln2_queue = []

        def ln2_pump(final=False):
            while ln2_queue:
                k = ln2_queue.pop(0)
                j_s1(k)
                if k >= 1:
                    j_s2(k - 1)
            if final:
                j_s2(NT - 1)

        steps = [(e_, tc) for e_ in range(NEXP) for tc in range(4)]
        load_expert(0)
        it = 0
        for i in range(NT):
            b = i % 2
            P.add("sp", lambda e, i=i, b=b: e.dma_start(out=ptile[b][:, 0:256], in_=p_d[i * 128:(i + 1) * 128, :]), writes=[("ptile", b)])
            for k in range(2):
                P.add("pe", lambda e, k=k, b=b: e.transpose(out=ps[:, 3072 + b * 512 + k * 128: 3072 + b * 512 + (k + 1) * 128], in_=ptile[b][:, k * 128:(k + 1) * 128], identity=ident),
                      reads=[("ptile", b), "ident"], writes=[("ps", 6 + b)])
            P.add("act", lambda e, b=b: e.copy(out=pT[b], in_=ps[:, 3072 + b * 512: 3072 + b * 512 + 256].rearrange("p (k n) -> p k n", k=2)),
                  reads=[], writes=[("pT", b), ("ps", 6 + b)])
            for hh in range(2):
                q = it % 2
                it += 1
                bg, bp = q * 2, q * 2 + 1
                for k in range(8):
                    P.add("pe", lambda e, k=k, i=i, hh=hh, bg=bg: e.matmul(bank(bg), lhsT=XT[:, k, i * 128:(i + 1) * 128], rhs=wpg_bf[:, k, hh * 512:(hh + 1) * 512], start=(k == 0), stop=(k == 7)),
                          reads=[("xT", i), ("wpg_bf", hh)], writes=[("ps", bg)])
                for k in range(2):
                    P.add("pe", lambda e, k=k, b=b, hh=hh, bp=bp: e.matmul(bank(bp), lhsT=pT[b][:, k, :], rhs=wpp_bf[:, k, hh * 512:(hh + 1) * 512], start=(k == 0), stop=(k == 1)),
                          reads=[("pT", b), "wpp_bf"], writes=[("ps", bp)])
                P.add("act", lambda e, q=q, bg=bg: e.activation(out=sgb[q], in_=bank(bg), func=AF.Sigmoid), reads=[("ps", bg)], writes=[("sgb", q)])
                P.add("dve", lambda e, q=q, bp=bp: e.tensor_tensor(out=sgb[q], in0=bank(bp), in1=sgb[q], op=ALU.mult), reads=[("ps", bp), ("sgb", q)], writes=[("sgb", q)])
                P.add("pool", lambda e, q=q, i=i, hh=hh: e.tensor_tensor(out=ACC[:, i, hh * 512:(hh + 1) * 512], in0=ACC[:, i, hh * 512:(hh + 1) * 512], in1=sgb[q], op=ALU.add),
                      reads=[("sgb", q), ("ACC", i)], writes=[("ACC", i)])
            for _ in range(3):
                if router_ops:
                    reng, rfn, rreads, rwrites = router_ops.pop(0)
                    P.add(reng, rfn, reads=rreads, writes=rwrites)
        while router_ops:
            reng, rfn, rreads, rwrites = router_ops.pop(0)
            P.add(reng, rfn, reads=rreads, writes=rwrites)
        if dbg:
            dbg_dump("cmb", R(76, 2), ["CMB"])
        if dbg:
            dbg_dump("acc2", R(110, 64), [("ACC", i) for i in range(NT)])
        for si in range(len(steps) + 1):
            if si < len(steps):
                gu_step(*steps[si])
            if si >= 1:
                down_step(*steps[si - 1])
                if si >= 2:
                    pe_, ptc = steps[si - 2]
                    if pe_ == NEXP - 1:
                        ln2_queue.extend(range(4 * ptc, 4 * ptc + 4))
                        ln2_pump()
            if si < len(steps):
                e_, tc = steps[si]
                if tc == 0 and e_ + 1 < NEXP:
                    load_expert(e_ + 1)
        phase("J")
        ln2_queue.extend(range(NT - 4, NT))
        ln2_pump(final=True)

        P.emit(sems, block)
    return nc, dbg_outs


def make_consts():
    j = np.arange(128)
    triU = (j[:, None] >= j[None, :]).astype(np.float32)
    ones = np.ones((128, 128), np.float32)
    mask01 = (j[:, None] < j[None, :]).astype(np.float32)
    rcs = np.zeros((4, 16), np.float32)
    for g, w in enumerate((2, 4, 8, 16)):
        rcs[g] = 1.0 / np.minimum(np.arange(1, 17), w)
    rcs = np.broadcast_to(rcs.reshape(1, 64), (128, 64))
    return np.ascontiguousarray(np.concatenate([triU, ones, mask01, rcs], axis=1).astype(np.float32))


def prep_shared(w_in, w_pool, pool_scale, w_pu, w_au, w_o, ln1_g, ln1_b, w_rg, b_rg, w_re, b_re,
                w_eg, w_eu, w_ed, w_pg, w_pp, ln2_g, ln2_b):
    f = lambda a: np.ascontiguousarray(np.asarray(a, dtype=np.float32))
    wr = np.concatenate([np.asarray(w_rg[0]), np.transpose(np.asarray(w_re[0]), (1, 0, 2)).reshape(D, 32)], axis=1)
    br = np.concatenate([np.asarray(b_rg[0]), np.asarray(b_re[0]).reshape(32)])
    return {
        "w_in": f(w_in[0]), "w_pool": f(w_pool[0]), "pscale": f(np.asarray(pool_scale[0]).reshape(4, 128).T),
        "w_pu": f(w_pu[0]), "w_au": f(w_au[0]), "w_o": f(w_o[0]), "ln1_g": f(ln1_g[0]), "ln1_b": f(ln1_b[0]),
        "wr": f(wr), "br": f(br),
        "w_eg": f(np.asarray(w_eg[0]).reshape(NEXP, D, 512)), "w_eu": f(np.asarray(w_eu[0]).reshape(NEXP, D, 512)),
        "w_ed": f(np.asarray(w_ed[0]).reshape(NEXP, 512, D)),
        "w_pg": f(w_pg[0]), "w_pp": f(w_pp[0]), "ln2_g": f(ln2_g[0]), "ln2_b": f(ln2_b[0]),
        "cst": make_consts(), "ident": np.eye(128, dtype=np.float32),
    }


def kernel(x, p, w_in, w_pool, pool_scale, w_pu, w_au, w_o, ln1_g, ln1_b,
           w_rg, b_rg, w_re, b_re, w_eg, w_eu, w_ed, w_pg, w_pp, ln2_g, ln2_b):
    x = np.asarray(x, dtype=np.float32)
    p = np.asarray(p, dtype=np.float32)
    shared = prep_shared(w_in, w_pool, pool_scale, w_pu, w_au, w_o, ln1_g, ln1_b, w_rg, b_rg, w_re, b_re,
                         w_eg, w_eu, w_ed, w_pg, w_pp, ln2_g, ln2_b)
    n = x.shape[0]
    nc, _ = build(False)
    in_maps = []
    for c in range(n):
        m = dict(shared)
        m["x"] = np.ascontiguousarray(x[c])
        m["p"] = np.ascontiguousarray(p[0, c])
        in_maps.append(m)
    res = run_bass_kernel_spmd(nc, in_maps, core_ids=list(range(n)))
    return np.stack([np.asarray(r["out"], dtype=np.float32) for r in res.results], axis=0)
```
